# Optimizing a Trainium2 kernel written in Bass

```python
import math
import jax, jax.numpy as jnp
from jax import lax
import numpy as np

D_MODEL = 1024
BATCH = 8
SEQ = 2048
DEPTH = 2

D_MIX = D_MODEL
D_ATTN = D_MIX // 2
D_LRU = D_MIX - D_ATTN
HEAD_DIM = 64
N_ATTN_HEADS = D_ATTN // HEAD_DIM
N_LRU_BLOCKS = 8
LRU_BLOCK = D_LRU // N_LRU_BLOCKS
CONV_WIDTH = 4
RG_C = 8.0
MOBA_BLOCK = 256
MOBA_TOPK = 3
Q_CHUNK = 32
D_IN = 4 * D_ATTN + 2 * D_LRU
EPS = 1e-6

kernel_name = "hybrid_moba_rglru_adaln"


def rmsnorm(x, w):
    xf = x.astype(jnp.float32)
    y = xf * lax.rsqrt(jnp.mean(xf * xf, axis=-1, keepdims=True) + EPS)
    return (y * w.astype(jnp.float32)).astype(x.dtype)


def alibi_slopes(n_heads):
    return jnp.exp2(-8.0 * (jnp.arange(n_heads, dtype=jnp.float32) + 1.0) / n_heads)


def moba_attention(q, k, v):
    B, S, H, Dh = q.shape
    n_blk = -(-S // MOBA_BLOCK)
    s_pad = n_blk * MOBA_BLOCK
    k_eff = min(MOBA_TOPK, n_blk)
    pad = ((0, 0), (0, s_pad - S), (0, 0), (0, 0))
    q = jnp.transpose(q, (0, 2, 1, 3))
    k = jnp.transpose(jnp.pad(k, pad), (0, 2, 1, 3))
    v = jnp.transpose(jnp.pad(v, pad), (0, 2, 1, 3))
    k_blocks = k.reshape(B, H, n_blk, MOBA_BLOCK, Dh)
    v_blocks = v.reshape(B, H, n_blk, MOBA_BLOCK, Dh)
    k_mean = jnp.mean(k_blocks.astype(jnp.float32), axis=3)
    scale = Dh ** -0.5
    slopes = alibi_slopes(H)[None, :, None, None]
    blk_ids = jnp.arange(n_blk)
    offs = jnp.arange(MOBA_BLOCK)
    b_ix = jnp.arange(B)[:, None, None, None]
    h_ix = jnp.arange(H)[None, :, None, None]
    n_sel = k_eff * MOBA_BLOCK

    def chunk(ci):
        q0 = ci * Q_CHUNK
        qb = q0 // MOBA_BLOCK
        qc = lax.dynamic_slice_in_dim(q, q0, Q_CHUNK, axis=2).astype(jnp.float32)
        q_pos = q0 + jnp.arange(Q_CHUNK)
        gate = jnp.einsum('bhqd,bhnd->bhqn', qc, k_mean)
        gate = jnp.where(blk_ids < qb, gate, -jnp.inf)
        _, sel = lax.top_k(gate, k_eff)
        valid = sel < qb
        ks = k_blocks[b_ix, h_ix, sel].astype(jnp.float32)
        vs = v_blocks[b_ix, h_ix, sel].astype(jnp.float32)
        k_pos_sel = sel[..., None] * MOBA_BLOCK + offs
        dist_sel = (q_pos[None, None, :, None, None] - k_pos_sel).astype(jnp.float32)
        s_sel = jnp.einsum('bhqd,bhqkpd->bhqkp', qc, ks) * scale - slopes[..., None] * dist_sel
        s_sel = jnp.where(valid[..., None], s_sel, -jnp.inf).reshape(B, H, Q_CHUNK, n_sel)
        k_own = lax.dynamic_slice_in_dim(k, qb * MOBA_BLOCK, MOBA_BLOCK, axis=2).astype(jnp.float32)
        v_own = lax.dynamic_slice_in_dim(v, qb * MOBA_BLOCK, MOBA_BLOCK, axis=2).astype(jnp.float32)
        k_pos_own = qb * MOBA_BLOCK + offs
        dist_own = q_pos[:, None] - k_pos_own[None, :]
        s_own = jnp.einsum('bhqd,bhpd->bhqp', qc, k_own) * scale - slopes * dist_own.astype(jnp.float32)
        s_own = jnp.where(dist_own >= 0, s_own, -jnp.inf)
        p = jax.nn.softmax(jnp.concatenate([s_sel, s_own], axis=-1), axis=-1)
        p_sel = p[..., :n_sel].reshape(B, H, Q_CHUNK, k_eff, MOBA_BLOCK)
        p_own = p[..., n_sel:]
        out = (jnp.einsum('bhqkp,bhqkpd->bhqd', p_sel, vs)
               + jnp.einsum('bhqp,bhpd->bhqd', p_own, v_own))
        return out.astype(q.dtype)

    outs = lax.map(chunk, jnp.arange(S // Q_CHUNK))
    return jnp.transpose(outs, (1, 0, 3, 2, 4)).reshape(B, S, H * Dh)


def rg_lru(xl, conv_w, conv_b, a_w, a_b, x_w, x_b, lam):
    B, S, C = xl.shape
    xc = lax.conv_general_dilated(
        xl, conv_w[:, None, :].astype(xl.dtype), window_strides=(1,),
        padding=[(CONV_WIDTH - 1, 0)], dimension_numbers=('NWC', 'WIO', 'NWC'),
        feature_group_count=C) + conv_b
    xg = xc.reshape(B, S, N_LRU_BLOCKS, LRU_BLOCK)
    r = jax.nn.sigmoid(jnp.einsum('bsgi,gij->bsgj', xg, a_w).reshape(B, S, C) + a_b)
    i = jax.nn.sigmoid(jnp.einsum('bsgi,gij->bsgj', xg, x_w).reshape(B, S, C) + x_b)
    log_a = -RG_C * r.astype(jnp.float32) * jax.nn.softplus(-lam.astype(jnp.float32))
    a = jnp.exp(log_a)
    b = jnp.sqrt(-jnp.expm1(2.0 * log_a)) * (i * xc).astype(jnp.float32)

    def combine(left, right):
        a_l, b_l = left
        a_r, b_r = right
        return a_l * a_r, a_r * b_l + b_r

    _, h = lax.associative_scan(combine, (a, b), axis=1)
    return h.astype(xl.dtype)


def hybrid_layer(x, c, norm_w, ada_w, ada_b, w_in, conv_w, conv_b, rg_a_w, rg_a_b,
                 rg_x_w, rg_x_b, rg_lambda, attn_out_norm_w, lru_out_norm_w, w_out):
    B, S, _ = x.shape
    mod = jax.nn.silu(c) @ ada_w + ada_b
    shift, scale, gate = jnp.split(mod[:, None, :], 3, axis=-1)
    h = rmsnorm(x, norm_w) * (1.0 + scale) + shift
    proj = h @ w_in
    q, k, v, z_attn, x_lru, z_lru = jnp.split(
        proj, [D_ATTN, 2 * D_ATTN, 3 * D_ATTN, 4 * D_ATTN, 4 * D_ATTN + D_LRU], axis=-1)
    hs = (B, S, N_ATTN_HEADS, HEAD_DIM)
    y_attn = moba_attention(q.reshape(hs), k.reshape(hs), v.reshape(hs))
    y_attn = rmsnorm(y_attn, attn_out_norm_w) * jax.nn.silu(z_attn)
    y_lru = rg_lru(x_lru, conv_w, conv_b, rg_a_w, rg_a_b, rg_x_w, rg_x_b, rg_lambda)
    y_lru = rmsnorm(y_lru, lru_out_norm_w) * jax.nn.silu(z_lru)
    y = jnp.concatenate([y_attn, y_lru], axis=-1) @ w_out
    return x + gate * y


def setup_inputs(seed: int = 0) -> dict:
    key = jax.random.key(seed)
    ks = jax.random.split(key, 20)
    f32 = jnp.float32
    L = DEPTH
    nrm = lambda k, shape, s: jax.random.normal(k, shape, f32) * s
    u = jax.random.uniform(ks[13], (L, D_LRU), f32, 0.9, 0.999)
    a0 = u ** (1.0 / RG_C)
    return {
        "x": jax.random.normal(ks[0], (BATCH, SEQ, D_MODEL), f32),
        "c": jax.random.normal(ks[1], (BATCH, D_MODEL), f32),
        "norm_w": 1.0 + nrm(ks[2], (L, D_MODEL), 0.05),
        "ada_w": nrm(ks[3], (L, D_MODEL, 3 * D_MODEL), D_MODEL ** -0.5),
        "ada_b": nrm(ks[4], (L, 3 * D_MODEL), 0.01),
        "w_in": nrm(ks[5], (L, D_MODEL, D_IN), D_MODEL ** -0.5),
        "conv_w": nrm(ks[6], (L, CONV_WIDTH, D_LRU), CONV_WIDTH ** -0.5),
        "conv_b": nrm(ks[7], (L, D_LRU), 0.01),
        "rg_a_w": nrm(ks[8], (L, N_LRU_BLOCKS, LRU_BLOCK, LRU_BLOCK), LRU_BLOCK ** -0.5),
        "rg_a_b": nrm(ks[9], (L, D_LRU), 0.01),
        "rg_x_w": nrm(ks[10], (L, N_LRU_BLOCKS, LRU_BLOCK, LRU_BLOCK), LRU_BLOCK ** -0.5),
        "rg_x_b": nrm(ks[11], (L, D_LRU), 0.01),
        "rg_lambda": jnp.log(a0) - jnp.log1p(-a0),
        "attn_out_norm_w": 1.0 + nrm(ks[14], (L, D_ATTN), 0.05),
        "lru_out_norm_w": 1.0 + nrm(ks[15], (L, D_LRU), 0.05),
        "w_out": nrm(ks[16], (L, D_MIX, D_MODEL), D_MIX ** -0.5),
        "final_norm_w": 1.0 + nrm(ks[17], (D_MODEL,), 0.05),
    }


def reference(x, c, norm_w, ada_w, ada_b, w_in, conv_w, conv_b, rg_a_w, rg_a_b, rg_x_w,
              rg_x_b, rg_lambda, attn_out_norm_w, lru_out_norm_w, w_out, final_norm_w):
    for l in range(DEPTH):
        x = hybrid_layer(x, c, norm_w[l], ada_w[l], ada_b[l], w_in[l], conv_w[l], conv_b[l],
                         rg_a_w[l], rg_a_b[l], rg_x_w[l], rg_x_b[l], rg_lambda[l],
                         attn_out_norm_w[l], lru_out_norm_w[l], w_out[l])
    return rmsnorm(x, final_norm_w)
```

```python
import contextlib
import numpy as np
import ml_dtypes
import concourse.bass as bass
import concourse.mybir as mybir
from concourse.bass_utils import run_bass_kernel_spmd

F32 = mybir.dt.float32
BF16 = mybir.dt.bfloat16
AF = mybir.ActivationFunctionType
ALU = mybir.AluOpType
AX = mybir.AxisListType

S_LEN = 2048
D = 1024
NT = 16
H = 8
EPS = 1e-6
NEG = -30000.0
ENGS = ["sync", "scalar", "vector", "gpsimd", "tensor"]


class _Stop(Exception):
    pass


class Sched:
    def __init__(self, nc, stack):
        self.nc = nc
        self.stack = stack
        self.prog = {e: [] for e in ENGS}
        self.cnt = {e: 0 for e in ENGS}
        self.sems = {}
        self.seen = {e: {} for e in ENGS}
        self.last_w = {}
        self.readers = {}
        self.dma_cnt = {}
        self.pending = {e: {} for e in ENGS}
        self.waited = set()
        self.final = {e: {} for e in ENGS}

    def sem(self, name):
        if name not in self.sems:
            self.sems[name] = self.stack.enter_context(self.nc.semaphore(name))
        return self.sems[name]

    def _collect(self, eng, reads, writes, extra=()):
        waits = dict(self.pending[eng])
        self.pending[eng] = {}

        def need(tok):
            if tok is None:
                return
            s, v = tok
            if v > waits.get(s, 0):
                waits[s] = v

        for k in reads:
            need(self.last_w.get(k))
        for k in writes:
            need(self.last_w.get(k))
            for s, v in self.readers.get(k, {}).items():
                need((s, v))
        for t in extra:
            need(t)
        wl = []
        for s, v in waits.items():
            if eng == "tensor" and s == "e_tensor":
                continue
            if self.seen[eng].get(s, 0) >= v:
                continue
            self.seen[eng][s] = v
            self.waited.add((s, v))
            wl.append((s, v))
        return wl

    def _commit(self, tok, reads, writes):
        for k in writes:
            self.last_w[k] = tok
            self.readers[k] = {}
        for k in reads:
            r = self.readers.setdefault(k, {})
            if tok[1] > r.get(tok[0], 0):
                r[tok[0]] = tok[1]

    def op(self, eng, fn, reads=(), writes=()):
        wl = self._collect(eng, reads, writes)
        self.cnt[eng] += 1
        tok = ("e_" + eng, self.cnt[eng])
        self.sem(tok[0])
        self.prog[eng].append((wl, fn, tok, False))
        self._commit(tok, reads, writes)
        return tok

    def dma(self, eng, stream, out, in_, reads=(), writes=(), **kw):
        n = self.dma_cnt.get(stream, 0)
        extra = [("d_" + stream, 16 * n)] if n > 0 else []
        wl = self._collect(eng, reads, writes, extra)
        self.dma_cnt[stream] = n + 1
        tok = ("d_" + stream, 16 * (n + 1))
        self.sem(tok[0])
        self.prog[eng].append((wl, (lambda e: e.dma_start(out=out, in_=in_, **kw)), tok, True))
        self._commit(tok, reads, writes)
        return tok

    def barrier(self):
        toks = {}
        for e in ENGS:
            if self.cnt[e] > 0:
                toks["e_" + e] = self.cnt[e]
        for s, n in self.dma_cnt.items():
            toks["d_" + s] = 16 * n
        for e in ENGS:
            for s, v in toks.items():
                if v > self.pending[e].get(s, 0):
                    self.pending[e][s] = v
        self.last_w = {}
        self.readers = {}

    def wait_at_end(self, eng, tok):
        self.final[eng][tok[0]] = max(self.final[eng].get(tok[0], 0), tok[1])
        self.waited.add(tok)

    def emit(self, eng, e):
        remap = {}
        run = 0
        for (wl, fn, tok, is_dma) in self.prog[eng]:
            if not is_dma and tok in self.waited:
                run += 1
                remap[tok[1]] = run
        self._remaps[eng] = remap

    def emit_all(self, block_engines):
        self._remaps = {}
        for eng in ENGS:
            self.emit(eng, None)

        def mapped(s, v):
            if s.startswith("e_"):
                return self._remaps[s[2:]][v]
            return v

        def run(eng, e):
            for (wl, fn, tok, is_dma) in self.prog[eng]:
                for (s, v) in wl:
                    e.wait_ge(self.sems[s], mapped(s, v))
                inst = fn(e)
                if is_dma:
                    inst.then_inc(self.sems[tok[0]], 16)
                elif tok in self.waited:
                    inst.then_inc(self.sems[tok[0]], 1)
            for s, v in self.pending[eng].items():
                pass
            for s, v in self.final[eng].items():
                e.wait_ge(self.sems[s], mapped(s, v))

        return run


def alibi_slopes():
    return [2.0 ** (-8.0 * (i + 1) / H) for i in range(H)]


def host_consts():
    sl = alibi_slopes()
    c = {}
    c["ident_bf"] = np.eye(128, dtype=np.float32).astype(ml_dtypes.bfloat16)
    c["ident_f"] = np.eye(128, dtype=np.float32)
    k = np.arange(128)[:, None]
    q = np.arange(128)[None, :]
    c["trimask"] = np.where(q >= k, 0.0, NEG).astype(np.float32).astype(ml_dtypes.bfloat16)
    bt = np.zeros((128, H, 17), np.float32)
    for h in range(H):
        for j in range(17):
            bt[:, h, j] = sl[h] * (np.arange(128) - 128.0 * (j - 1))
    c["bias_tab"] = bt.reshape(128, H * 17)
    qi = (np.arange(S_LEN) % 256).astype(np.float32)
    c["arow"] = np.stack([-8.0 * sl[h] * qi for h in range(H)]).astype(np.float32).astype(ml_dtypes.bfloat16)
    c["karow"] = np.stack([8.0 * sl[h] * qi for h in range(H)]).astype(np.float32).astype(ml_dtypes.bfloat16)
    qbi = (np.arange(S_LEN) // 256).astype(np.float32)
    c["arow_hi"] = np.stack([-8.0 * sl[h] * 256.0 * qbi for h in range(H)]).astype(np.float32).astype(ml_dtypes.bfloat16)
    c["karow_hi"] = np.stack([8.0 * sl[h] * 256.0 * qbi for h in range(H)]).astype(np.float32).astype(ml_dtypes.bfloat16)
    kr = np.zeros((9, S_LEN), np.float32)
    for n in range(8):
        kr[n, n * 256:(n + 1) * 256] = 1.0
    kr[8, :] = 1.0
    c["krows"] = kr.astype(ml_dtypes.bfloat16)
    gm = np.zeros((128, 8, 8), np.float32)
    for j in range(8):
        qb = 4 + j // 2
        gm[:, j, qb:] = -1e30
    c["gmask"] = gm.reshape(128, 64)
    vn = np.zeros((128, 8, 8), np.float32)
    for j in range(8):
        qb = 4 + j // 2
        vn[:, j, :qb] = NEG
    c["vneg"] = vn.reshape(128, 64)
    c["ones_bf"] = np.ones((128, 128), np.float32).astype(ml_dtypes.bfloat16)
    return c


def col_layout(L):
    off = {}
    n = 0

    def add(name, w):
        nonlocal n
        off[name] = (n, w)
        n += w

    add("cT", 8)
    for l in range(L):
        add(("normw", l), 8)
        add(("convw", l), 16)
        add(("convb", l), 4)
        add(("rab", l), 4)
        add(("rxb", l), 4)
        add(("lam", l), 4)
        add(("anw", l), 4)
        add(("lnw", l), 4)
    return off, n


def pack_cols(inp, b, L):
    off, n = col_layout(L)
    cols = np.zeros((128, n), np.float32)

    def put(name, arr):
        o, w = off[name]
        assert arr.shape == (128, w), (name, arr.shape)
        cols[:, o:o + w] = arr

    put("cT", inp["c"][b].reshape(8, 128).T)
    for l in range(L):
        put(("normw", l), inp["norm_w"][l].reshape(8, 128).T)
        cw = inp["conv_w"][l]
        put(("convw", l), cw.reshape(4, 4, 128).transpose(2, 1, 0).reshape(128, 16))
        for nm, key in (("convb", "conv_b"), ("rab", "rg_a_b"), ("rxb", "rg_x_b"), ("lam", "rg_lambda"),
                        ("anw", "attn_out_norm_w"), ("lnw", "lru_out_norm_w")):
            put((nm, l), inp[key][l].reshape(4, 128).T)
    return cols


def blockdiag(w):
    L = w.shape[0]
    out = np.zeros((L, 4, 128, 128), np.float32)
    for c in range(4):
        out[:, c, 0:64, 0:64] = w[:, 2 * c]
        out[:, c, 64:128, 64:128] = w[:, 2 * c + 1]
    return out


def build(layers, final, L_total, dbg=()):
    nc = bass.Bass("TRN2", target_bir_lowering=False)
    coff, ncol = col_layout(L_total)
    dr = {}

    def din(name, shape, dt=F32):
        dr[name] = nc.dram_tensor(name, list(shape), dt, kind="ExternalInput").ap()
        return dr[name]

    x_d = din("x", [S_LEN, D])
    cols_d = din("cols", [128, ncol])
    adaw_d = din("ada_w", [L_total, D, 3 * D])
    adab_d = din("ada_b", [L_total, 3 * D])
    win_d = din("w_in", [L_total, D, 3 * D])
    wout_d = din("w_out", [L_total, D, D])
    raw_d = din("rg_a_bd", [L_total, 4, 128, 128])
    rxw_d = din("rg_x_bd", [L_total, 4, 128, 128])
    fnw_d = din("final_norm_w", [D])
    din("conv_b", [L_total, 512])
    identbf_d = din("ident_bf", [128, 128], BF16)
    identf_d = din("ident_f", [128, 128])
    trimask_d = din("trimask", [128, 128], BF16)
    biastab_d = din("bias_tab", [128, H * 17])
    arow_d = din("arow", [H, S_LEN], BF16)
    krows_d = din("krows", [9, S_LEN], BF16)
    karow_d = din("karow", [H, S_LEN], BF16)
    arowhi_d = din("arow_hi", [H, S_LEN], BF16)
    karowhi_d = din("karow_hi", [H, S_LEN], BF16)
    gmask_d = din("gmask", [128, 64])
    vneg_d = din("vneg", [128, 64])
    onesbf_d = din("ones_bf", [128, 128], BF16)
    out_d = nc.dram_tensor("out", [S_LEN, D], F32, kind="ExternalOutput").ap()
    dbg_d = {}

    UW = 22784
    with contextlib.ExitStack() as st:
        def sb(name, shape, dt):
            return st.enter_context(nc.sbuf_tensor(name, list(shape), dt))

        x_sb = sb("x_sb", [128, NT, D], F32)
        hT = sb("hT", [128, 8, S_LEN], BF16)
        cc = sb("cc", [128, 8, S_LEN], BF16)
        stg = [sb("stg0", [128, 4096], BF16), sb("stg1", [128, 4096], BF16)]
        mod_bc = sb("mod_bc", [128, 3 * D], F32)
        U = sb("U", [128, UW], BF16)
        Uf = U.bitcast(F32)
        ident_bf = sb("ident_bf_s", [128, 128], BF16)
        ident_f = sb("ident_f_s", [128, 128], F32)
        trimask = sb("trimask_s", [128, 128], BF16)
        bias_tab = sb("bias_tab_s", [128, H * 17], F32)
        gmask = sb("gmask_s", [128, 64], F32)
        vneg = sb("vneg_s", [128, 64], F32)
        ones_bf = sb("ones_bf_s", [128, 128], BF16)
        ones4 = sb("ones4_s", [1, 512], BF16)
        cols = sb("cols_s", [128, ncol], F32)
        sm = sb("small", [128, 512], F32)
        ps = [st.enter_context(nc.psum_tensor("ps%d" % i, [128, 512], F32)) for i in range(8)]
        psb = [p.bitcast(BF16) for p in ps]

        S = Sched(nc, st)

        def col(name, j0=0, w=None):
            o, ww = coff[name]
            if w is None:
                w = ww - j0
            return cols[:, o + j0:o + j0 + w]

        smo = {}
        smn = [0]

        def smc(name, w):
            if name not in smo:
                smo[name] = (smn[0], w)
                smn[0] += w
                assert smn[0] <= 512
            o, ww = smo[name]
            return sm[:, o:o + ww]

        onesrow = ones4[0:1, :]
        S.dma("sync", "c8", onesrow, krows_d[8:9, 0:512], writes=[("onesrow",)])
        qcol = smc("qcol", 1)
        mhalf = smc("mhalf", 1)
        S.op("vector", lambda e: e.memset(mhalf, -0.5), writes=[("mhalf",)])
        S.op("vector", lambda e: e.memset(qcol, 0.25), writes=[("qcol",)])
        S.dma("sync", "c0", cols[:, :], cols_d, writes=[("cols",)])
        S.dma("sync", "c1", ident_bf[:, :], identbf_d, writes=[("ident_bf",)])
        S.dma("sync", "c2", ident_f[:, :], identf_d, writes=[("ident_f",)])
        S.dma("sync", "c3", trimask[:, :], trimask_d, writes=[("trimask",)])
        S.dma("sync", "c4", bias_tab[:, :], biastab_d, writes=[("bias_tab",)])
        S.dma("sync", "c5", gmask[:, :], gmask_d, writes=[("gmask",)])
        S.dma("sync", "c7", vneg[:, :], vneg_d, writes=[("vneg",)])
        S.dma("sync", "c6", ones_bf[:, :], onesbf_d, writes=[("ones_bf",)])

        dbg_cnt = [0]

        def dump(name, ap, shape, dt, reads):
            if name not in dbg:
                return
            t = nc.dram_tensor("dbg_" + name, list(shape), dt, kind="ExternalOutput").ap()
            dbg_d[name] = t
            tok = S.dma("sync", "dbg%d" % dbg_cnt[0], t, ap, reads=reads)
            dbg_cnt[0] += 1
            S.wait_at_end("sync", tok)


        dparts = {}

        def dump_part(name, c, hf, ap, reads):
            if name not in dparts:
                dparts[name] = nc.dram_tensor("dbg_" + name, [128, 4, S_LEN], F32, kind="ExternalOutput").ap()
                dbg_d[name] = dparts[name]
            tok = S.dma("sync", "dbg%d" % dbg_cnt[0], dparts[name][:, c, hf * 1024:(hf + 1) * 1024], ap, reads=reads)
            dbg_cnt[0] += 1
            S.wait_at_end("sync", tok)

        ssqx = smc("ssqx", 16)
        rstdx = smc("rstdx", 16)

        def x_stats(tt, junk_ap):
            S.op("scalar", lambda e: e.activation(
                out=junk_ap, in_=x_sb[:, tt, :], func=AF.Square, accum_out=ssqx[:, tt:tt + 1]),
                reads=[("x", tt)], writes=[("junkx",), ("ssqx", tt)])
            S.op("gpsimd", lambda e: e.tensor_scalar(
                out=rstdx[:, tt:tt + 1], in0=ssqx[:, tt:tt + 1], scalar1=1.0 / D, scalar2=EPS, op0=ALU.mult, op1=ALU.add),
                reads=[("ssqx", tt)], writes=[("rstdx", tt)])
            S.op("gpsimd", lambda e: e.tensor_tensor(
                out=rstdx[:, tt:tt + 1], in0=rstdx[:, tt:tt + 1], in1=mhalf, op=ALU.pow),
                reads=[("rstdx", tt), ("mhalf",)], writes=[("rstdx", tt)])

        def mod_prep(scoff, aboff):
            th = smc("th", 8)
            sc = smc("sc", 8)
            cT = col("cT")
            S.op("scalar", lambda e: e.activation(out=th, in_=cT, func=AF.Tanh, scale=0.5),
                 reads=[("cols",)], writes=[("th",)])
            S.op("vector", lambda e: e.scalar_tensor_tensor(
                out=sc, in0=th, scalar=1.0, in1=cT, op0=ALU.add, op1=ALU.mult),
                reads=[("th",), ("cols",)], writes=[("sc",)])
            S.op("vector", lambda e: e.tensor_scalar(out=sc, in0=sc, scalar1=0.5, scalar2=None, op0=ALU.mult),
                 reads=[("sc",)], writes=[("sc",)])
            screp = U[:, scoff:scoff + 1024].rearrange("p (k m) -> p k m", k=8)
            for k in range(8):
                S.op("vector", lambda e, k=k: e.tensor_scalar(
                    out=screp[:, k, :], in0=ones_bf[:, :], scalar1=sc[:, k:k + 1], scalar2=None, op0=ALU.mult),
                    reads=[("sc",), ("ones_bf",)], writes=[("screp", k)])
            return screp, aboff

        def mod_bufs(in_u):
            if in_u:
                return [U[:, 0:4096], U[:, 4096:8192]], "ustg"
            return [stg[0][:, 0:4096], stg[1][:, 0:4096]], "stg"

        def mod_dma(l_, ng, in_u=False):
            adaw_v = adaw_d[l_].rearrange("(k p) c -> p k c", p=128)
            bufs, kn = mod_bufs(in_u)
            sgv = bufs[ng % 2].rearrange("p (k c) -> p k c", k=8)
            S.dma("gpsimd", "%s%d" % (kn, ng % 2), sgv, adaw_v[:, :, ng * 512:(ng + 1) * 512],
                  writes=[(kn, ng % 2), ("adaload", ng)])

        def mod_mm(l_, ng, mprep, in_u=False):
            screp, aboff = mprep
            bufs, kn = mod_bufs(in_u)
            if ng == 0:
                S.dma("gpsimd", "adab", U[0:1, aboff:aboff + 3 * D], adab_d[l_:l_ + 1, :], writes=[("adab",)],
                      max_dma_last_dim=2048)
            sgv = bufs[ng % 2].rearrange("p (k c) -> p k c", k=8)
            pt = ps[4 + ng % 2]
            pk = ("ps", 4 + ng % 2)
            for k in range(8):
                S.op("tensor", lambda e, k=k: e.matmul(
                    pt[:, :], lhsT=screp[:, k, :], rhs=sgv[:, k, :], start=(k == 0), stop=False),
                    reads=[("screp", k), (kn, ng % 2)], writes=[pk])
            S.op("tensor", lambda e: e.matmul(
                pt[:, :], lhsT=ones_bf[0:1, :], rhs=U[0:1, aboff + ng * 512:aboff + (ng + 1) * 512], start=False, stop=True),
                reads=[("ones_bf",), ("adab",)], writes=[pk])
            S.op("scalar", lambda e: e.activation(
                out=mod_bc[:, ng * 512:(ng + 1) * 512], in_=pt[:, :], func=AF.Copy),
                reads=[pk], writes=[("mod", ng)])

        first = True

        def stop_at(name):
            if name in dbg:
                raise _Stop()

        try:
          for li, l in enumerate(layers):
              last_layer = (li == len(layers) - 1)
              if li == 0:
                  mprep = mod_prep(0, 9216)
                  for ng in range(4):
                      mod_dma(l, ng)
                      mod_mm(l, ng, mprep)
                  mod_dma(l, 4)
                  mod_dma(l, 5)
              tmpd = Uf[:, 1024:1024 + 2048].rearrange("p (j m) -> p j m", j=16)
              identb = bass.AP(ident_f, 0, [[128, 128], [0, 16], [1, 128]])
              S.op("vector", lambda e: e.tensor_tensor(
                  out=tmpd, in0=mod_bc[:, 0:2048].rearrange("p (j m) -> p j m", j=16), in1=identb, op=ALU.mult),
                  reads=[("mod", g) for g in range(4)] + [("ident_f",)], writes=[("tmpd",)])
              col16 = smc("col16", 16)
              S.op("vector", lambda e: e.tensor_reduce(out=col16, in_=tmpd, axis=AX.X, op=ALU.add),
                   reads=[("tmpd",)], writes=[("col16",)])
              s1 = smc("s1", 8)
              S.op("vector", lambda e, l=l: e.scalar_tensor_tensor(
                  out=s1, in0=col16[:, 8:16], scalar=1.0, in1=col(("normw", l)), op0=ALU.add, op1=ALU.mult),
                  reads=[("col16",), ("cols",)], writes=[("s1",)])
              dump("mod%d" % l, mod_bc[:, :], [128, 3 * D], F32, [("mod", g) for g in range(6)])
              dump("s1_%d" % l, s1, [128, 8], F32, [("s1",)])

              junk = U[:, 6144:6144 + 1024]
              if li == 0:
                  for tt in range(NT):
                      if first:
                          S.dma("sync", "xin%d" % (tt % 4), x_sb[:, tt, :], x_d[tt * 128:(tt + 1) * 128, :],
                                reads=[("adaload", 2)], writes=[("x", tt)])
                      x_stats(tt, junk)
              xn = [U[:, 7168:7168 + 1024], U[:, 8192:8192 + 1024]]
              tmpf = [Uf[:, 6144:7168], Uf[:, 7168:8192]]
              s1_b = bass.AP(s1.tensor, s1.offset, [[s1.ap[0][0], 128], [1, 8], [0, 128]])
              sh_b = bass.AP(col16.tensor, col16.offset, [[col16.ap[0][0], 128], [1, 8], [0, 128]])
              for tt in range(NT):
                  xb = xn[tt % 2]
                  S.op("scalar", lambda e, tt=tt, xb=xb: e.activation(
                      out=xb, in_=x_sb[:, tt, :], func=AF.Copy, scale=rstdx[:, tt:tt + 1]),
                      reads=[("x", tt), ("rstdx", tt)], writes=[("xn", tt % 2)])
                  pb = psb[2 + tt % 2]
                  for k in range(8):
                      S.op("tensor", lambda e, k=k, xb=xb, pb=pb: e.transpose(
                          out=pb[:, k * 128:(k + 1) * 128], in_=xb[:, k * 128:(k + 1) * 128], identity=ident_bf[:, :]),
                          reads=[("xn", tt % 2), ("ident_bf",)], writes=[("ps", 2 + tt % 2)])
                  tf = tmpf[tt % 2].rearrange("p (k m) -> p k m", k=8)
                  S.op("vector", lambda e, pb=pb, tf=tf: e.tensor_tensor(
                      out=tf, in0=pb[:, 0:1024].rearrange("p (k m) -> p k m", k=8), in1=s1_b, op=ALU.mult),
                      reads=[("ps", 2 + tt % 2), ("s1",)], writes=[("tmpf", tt % 2)])
                  S.op("vector", lambda e, tt=tt, tf=tf: e.tensor_tensor(
                      out=hT[:, :, tt * 128:(tt + 1) * 128], in0=tf, in1=sh_b, op=ALU.add),
                      reads=[("tmpf", tt % 2), ("col16",)], writes=[("hT", tt)])
                  if li == 0 and tt in (5, 10):
                      mod_mm(l, 4 if tt == 5 else 5, mprep)
              dump("hT%d" % l, hT[:, :, :], [128, 8, S_LEN], BF16, [("hT", t) for t in range(NT)])
              dump("xin%d" % l, x_sb[:, :, :], [128, NT, D], F32, [("x", t) for t in range(NT)])
              dump("c16_%d" % l, col16, [128, 16], F32, [("col16",)])
              first = False
              if "stopP0" in dbg:
                  S.barrier()
                  break
              TU = 512
              Abig = Uf[:, 0:2048]
              SVbig = Uf[:, 2048:4096]
              Mbig = Uf[:, 4096:6144]
              szls = [stg[0][:, 2048:4096], stg[1][:, 2048:4096]]
              hsq = mod_bc.bitcast(BF16)[:, 0:2048]
              xlb = [U[:, 14336:14336 + 516], U[:, 14852:14852 + 516]]
              xcbs = [U[:, 15368:15880], U[:, 15880:16392]]
              rts = [Uf[:, 8196:8708], Uf[:, 8708:9220]]
              itv = Uf[:, 9220:9732]
              tzb = Uf[:, 10500:11012]
              dgs = [U[:, 19464:19976], U[:, 19976:20488]]
              smb = sm.bitcast(BF16)
              wabs = [smb[:, 700:828], smb[:, 828:956]]
              wxbs = [U[:, 20488:20616], U[:, 20616:20744]]
              cbrow = [U[0:1, 20744:20872], U[0:1, 20872:21000]]
              lam = col(("lam", l))
              e1 = smc("e1", 4)
              nsp = smc("nsp", 4)
              hnsp = smc("hnsp", 4)
              habh = smc("habh", 4)
              hxbh = smc("hxbh", 4)
              lnwh = smc("lnwh", 4)
              anwh = smc("anwh", 4)
              ssql = smc("ssql", 16)
              hcar = smc("hcar", 1)
              S.op("scalar", lambda e, lam=lam: e.activation(out=e1, in_=lam, func=AF.Exp, scale=-1.0),
                   reads=[("cols",)], writes=[("e1",)])
              S.op("scalar", lambda e: e.activation(out=e1, in_=e1, func=AF.Ln, bias=1.0),
                   reads=[("e1",)], writes=[("e1",)])
              S.op("vector", lambda e: e.tensor_scalar(out=nsp, in0=e1, scalar1=-8.0, scalar2=None, op0=ALU.mult),
                   reads=[("e1",)], writes=[("nsp",)])
              S.op("vector", lambda e: e.tensor_scalar(out=hnsp, in0=e1, scalar1=-4.0, scalar2=None, op0=ALU.mult),
                   reads=[("e1",)], writes=[("hnsp",)])
              for (dst, srcn) in ((habh, "rab"), (hxbh, "rxb"), (lnwh, "lnw"), (anwh, "anw")):
                  S.op("vector", lambda e, dst=dst, srcn=srcn, l=l: e.tensor_scalar(
                      out=dst, in0=col((srcn, l)), scalar1=0.5, scalar2=None, op0=ALU.mult),
                      reads=[("cols",)], writes=[("halfcols", srcn)])
              dump("nsp%d" % l, nsp, [128, 4], F32, [("nsp",)])
              win_v = win_d[l].rearrange("(k p) c -> p k c", p=128)
              convb_d2 = dr["conv_b"]

              def lru_load(c):
                  sg = stg[c % 2]
                  wl_ = sg[:, 0:2048].rearrange("p (k c) -> p k c", k=8)
                  S.dma("gpsimd", "stg%d" % (c % 2), wl_[:, :, 0:128], win_v[:, :, 2048 + c * 128:2048 + (c + 1) * 128],
                        writes=[("stg", c % 2)])
                  S.dma("gpsimd", "stgb%d" % (c % 2), wl_[:, :, 128:256], win_v[:, :, 2560 + c * 128:2560 + (c + 1) * 128],
                        writes=[("stgz", c % 2), ("stg", c % 2)])
                  S.dma("gpsimd", "wab%d" % (c % 2), wabs[c % 2], raw_d[l, c], writes=[("wab", c % 2)])
                  S.dma("gpsimd", "wxb%d" % (c % 2), wxbs[c % 2], rxw_d[l, c], writes=[("wxb", c % 2)])
                  S.dma("gpsimd", "cbr%d" % (c % 2), cbrow[c % 2], convb_d2[l:l + 1, c * 128:(c + 1) * 128],
                        writes=[("cbrow", c % 2)])
                  cw = col(("convw", l), c * 4, 4)
                  for j in range(4):
                      S.op("vector", lambda e, j=j, cw=cw, c=c: e.tensor_scalar(
                          out=dgs[c % 2][:, j * 128:(j + 1) * 128], in0=ident_bf[:, :], scalar1=cw[:, j:j + 1], scalar2=None,
                          op0=ALU.mult),
                          reads=[("ident_bf",), ("cols",)], writes=[("dg", c % 2)])

              premm = set()

              def part1_mm(c, t):
                  b = (4 * c + t) % 2
                  wl_ = stg[c % 2][:, 0:2048].rearrange("p (k c) -> p k c", k=8)
                  pxl, pzl = ps[b], ps[2 + b]
                  kxl, kzl = ("ps", b), ("ps", 2 + b)
                  hkeys = [("hT", 4 * t + j) for j in range(4)]
                  for k in range(8):
                      S.op("tensor", lambda e, k=k: e.matmul(
                          pxl[:, :], lhsT=wl_[:, k, 0:128], rhs=hT[:, k, t * TU:(t + 1) * TU], start=(k == 0), stop=(k == 7)),
                          reads=hkeys + [("stg", c % 2)], writes=[kxl])
                  for k in range(8):
                      S.op("tensor", lambda e, k=k: e.matmul(
                          pzl[:, :], lhsT=wl_[:, k, 128:256], rhs=hT[:, k, t * TU:(t + 1) * TU], start=(k == 0), stop=(k == 7)),
                          reads=hkeys + [("stgz", c % 2)], writes=[kzl])

              def part1(c, t):
                  u = 4 * c + t
                  b = u % 2
                  wl_ = stg[c % 2][:, 0:2048].rearrange("p (k c) -> p k c", k=8)
                  xl_ = xlb[b]
                  pxl, pzl, pcv = ps[b], ps[2 + b], ps[4 + b]
                  kxl, kzl, kcv = ("ps", b), ("ps", 2 + b), ("ps", 4 + b)
                  hkeys = [("hT", 4 * t + j) for j in range(4)]
                  if (c, t) not in premm:
                      part1_mm(c, t)
                  if t == 0:
                      S.op("vector", lambda e: e.memset(xl_[:, 0:3], 0.0), writes=[("xlb", b, "h")])
                  else:
                      S.op("vector", lambda e: e.tensor_copy(out=xl_[:, 0:3], in_=xlb[1 - b][:, TU:TU + 3]),
                           reads=[("xlb", 1 - b)], writes=[("xlb", b, "h")])
                  S.op("vector", lambda e: e.tensor_copy(out=xl_[:, 3:3 + TU], in_=pxl[:, :]),
                       reads=[kxl], writes=[("xlb", b)])
                  S.op("scalar", lambda e: e.activation(out=tzb, in_=pzl[:, :], func=AF.Tanh, scale=0.5),
                       reads=[kzl], writes=[("tz",)])
                  S.op("vector", lambda e: e.scalar_tensor_tensor(
                      out=szls[c % 2][:, t * TU:(t + 1) * TU], in0=tzb, scalar=1.0, in1=pzl[:, :], op0=ALU.add, op1=ALU.mult),
                      reads=[("tz",), kzl], writes=[("szl", c % 2, t)])
                  for j in range(4):
                      S.op("tensor", lambda e, j=j: e.matmul(
                          pcv[:, :], lhsT=dgs[c % 2][:, j * 128:(j + 1) * 128], rhs=xl_[:, j:j + TU], start=(j == 0), stop=False),
                          reads=[("dg", c % 2), ("xlb", b), ("xlb", b, "h")], writes=[kcv])
                  S.op("tensor", lambda e: e.matmul(
                      pcv[:, :], lhsT=cbrow[c % 2], rhs=ones_bf[0:1, 0:TU] if False else onesrow, start=False, stop=True),
                      reads=[("cbrow", c % 2), ("onesrow",)], writes=[kcv])
                  S.op("scalar", lambda e: e.activation(out=xcbs[b], in_=pcv[:, :], func=AF.Copy),
                       reads=[kcv], writes=[("xcb", b)])

              def part2(c, t):
                  u = 4 * c + t
                  b = u % 2
                  pcv, kcv = ps[4 + b], ("ps", 4 + b)
                  rt_ = rts[b]
                  S.op("tensor", lambda e: e.matmul(ps[6][:, :], lhsT=wabs[c % 2], rhs=xcbs[b], start=True, stop=True),
                       reads=[("wab", c % 2), ("xcb", b)], writes=[("ps", 6)])
                  S.op("tensor", lambda e: e.matmul(ps[7][:, :], lhsT=wxbs[c % 2], rhs=xcbs[b], start=True, stop=True),
                       reads=[("wxb", c % 2), ("xcb", b)], writes=[("ps", 7)])
                  S.op("scalar", lambda e: e.activation(
                      out=rt_, in_=ps[6][:, :], func=AF.Tanh, scale=0.5, bias=habh[:, c:c + 1]),
                      reads=[("ps", 6), ("halfcols", "rab")], writes=[("rt", b)])
                  S.op("scalar", lambda e: e.activation(
                      out=itv, in_=ps[7][:, :], func=AF.Tanh, scale=0.5, bias=hxbh[:, c:c + 1]),
                      reads=[("ps", 7), ("halfcols", "rxb")], writes=[("itv",)])
                  S.op("scalar", lambda e: e.activation(
                      out=Abig[:, t * TU:(t + 1) * TU], in_=rt_, func=AF.Exp, scale=hnsp[:, c:c + 1], bias=hnsp[:, c:c + 1]),
                      reads=[("rt", b), ("hnsp",)], writes=[("A", t)])
                  S.op("scalar", lambda e: e.activation(
                      out=SVbig[:, t * TU:(t + 1) * TU], in_=rt_, func=AF.Exp, scale=nsp[:, c:c + 1], bias=nsp[:, c:c + 1]),
                      reads=[("rt", b), ("nsp",)], writes=[("SV", t)])
                  S.op("vector", lambda e: e.scalar_tensor_tensor(
                      out=Mbig[:, t * TU:(t + 1) * TU], in0=itv, scalar=1.0, in1=pcv[:, :], op0=ALU.add, op1=ALU.mult),
                      reads=[("itv",), kcv], writes=[("M", t)])

              allk = lambda nm: [(nm, t) for t in range(4)]

              def stage_b(c):
                  for hf in range(2):
                      sl_ = slice(hf * 1024, (hf + 1) * 1024)
                      S.op("scalar", lambda e, sl_=sl_: e.activation(
                          out=SVbig[:, sl_], in_=SVbig[:, sl_], func=AF.Sqrt, scale=-0.25, bias=qcol),
                          reads=[("SV", 2 * hf), ("SV", 2 * hf + 1), ("qcol",)], writes=[("SV", 2 * hf), ("SV", 2 * hf + 1)])

              def stage_c(c, hf):
                  sl_ = slice(hf * 1024, (hf + 1) * 1024)
                  ks = lambda nm: [(nm, 2 * hf), (nm, 2 * hf + 1)]
                  S.op("vector", lambda e: e.tensor_tensor(
                      out=Mbig[:, sl_], in0=SVbig[:, sl_], in1=Mbig[:, sl_], op=ALU.mult),
                      reads=ks("SV") + ks("M"), writes=ks("M"))
                  if hf == 0:
                      S.op("vector", lambda e: e.tensor_tensor_scan(
                          out=SVbig[:, sl_], data0=Abig[:, sl_], data1=Mbig[:, sl_], initial=0.0,
                          op0=ALU.mult, op1=ALU.add),
                          reads=ks("A") + ks("M") + ks("SV"), writes=ks("SV"))
                      S.op("vector", lambda e: e.tensor_copy(out=hcar, in_=SVbig[:, 1023:1024]),
                           reads=ks("SV"), writes=[("hcar",)])
                  else:
                      S.op("vector", lambda e: e.tensor_tensor_scan(
                          out=SVbig[:, sl_], data0=Abig[:, sl_], data1=Mbig[:, sl_], initial=hcar,
                          op0=ALU.mult, op1=ALU.add),
                          reads=ks("A") + ks("M") + ks("SV") + [("hcar",)], writes=ks("SV"))
                  S.op("vector", lambda e: e.scalar_tensor_tensor(
                      out=cc[:, 4 + c, sl_], in0=SVbig[:, sl_], scalar=lnwh[:, c:c + 1], in1=szls[c % 2][:, sl_],
                      op0=ALU.mult, op1=ALU.mult),
                      reads=ks("SV") + [("szl", c % 2, 2 * hf), ("szl", c % 2, 2 * hf + 1), ("halfcols", "lnw")],
                      writes=[("cc", 4 + c, 2 * hf), ("cc", 4 + c, 2 * hf + 1)])
                  if hf == 1 and ("hl%d" % l) in dbg:
                      dump_part("hl%d" % l, c, 0, SVbig[:, 0:1024], allk("SV"))
                      dump_part("hl%d" % l, c, 1, SVbig[:, 1024:2048], allk("SV"))

              def stage_sq(c, hf):
                  sl_ = slice(hf * 1024, (hf + 1) * 1024)
                  S.op("scalar", lambda e: e.activation(out=hsq[:, sl_], in_=SVbig[:, sl_], func=AF.Square),
                       reads=[("SV", 2 * hf), ("SV", 2 * hf + 1)], writes=[("hsq", hf)])

              def stage_ssq(c):
                  for t16 in range(16):
                      S.op("tensor", lambda e, t16=t16: e.matmul(
                          ps[6][:, t16:t16 + 1], lhsT=hsq[:, t16 * 128:(t16 + 1) * 128], rhs=ones_bf[:, 0:1],
                          start=True, stop=True),
                          reads=[("hsq", 0), ("hsq", 1), ("ones_bf",)], writes=[("ps", 6)])
                  if c == 0:
                      S.op("vector", lambda e: e.tensor_copy(out=ssql, in_=ps[6][:, 0:16]),
                           reads=[("ps", 6)], writes=[("ssql", 0), ("ssql", 1)])
                  else:
                      S.op("vector", lambda e: e.tensor_tensor(out=ssql, in0=ssql, in1=ps[6][:, 0:16], op=ALU.add),
                           reads=[("ps", 6), ("ssql", 0), ("ssql", 1)], writes=[("ssql", 0), ("ssql", 1)])

              Vt = U[:, 14336:22656].rearrange("p (t h d) -> p t h d", t=16, h=8)
              lru_alias = ([("xlb", b_) for b_ in (0, 1)] + [("xlb", b_, "h") for b_ in (0, 1)] + [("xcb", b_) for b_ in (0, 1)]
                           + [("rt", b_) for b_ in (0, 1)] + [("itv",)] + [("dg", b_) for b_ in (0, 1)]
                           + [("wxb", b_) for b_ in (0, 1)] + [("cbrow", b_) for b_ in (0, 1)] + [("tz",)])

              def v_proj():
                  wv = stg[0][:, 0:4096].rearrange("p (k c) -> p k c", k=8)
                  S.op("gpsimd", lambda e: e.memset(Vt[:, :, :, 64:65], 1.0), writes=[("Vones",)] + lru_alias)
                  for tt in range(NT):
                      pv = ps[tt % 6]
                      for k in range(8):
                          S.op("tensor", lambda e, k=k, tt=tt, pv=pv: e.matmul(
                              pv[:, :], lhsT=hT[:, k, tt * 128:(tt + 1) * 128], rhs=wv[:, k, :], start=(k == 0), stop=(k == 7)),
                              reads=[("hT", tt), ("stg", 0)], writes=[("ps", tt % 6)])
                      S.op("scalar" if tt % 2 else "vector", (lambda e, tt=tt, pv=pv: e.activation(
                          out=Vt[:, tt, :, 0:64], in_=pv[:, :].rearrange("p (h d) -> p h d", h=8), func=AF.Copy)) if tt % 2 else
                          (lambda e, tt=tt, pv=pv: e.tensor_copy(
                              out=Vt[:, tt, :, 0:64], in_=pv[:, :].rearrange("p (h d) -> p h d", h=8))),
                          reads=[("ps", tt % 6)], writes=[("V", tt)] + lru_alias)

              units = [(c, t) for c in range(4) for t in range(4)]
              lru_load(0)
              lru_load(1)
              part1_mm(0, 0)
              part1_mm(0, 1)
              premm.update([(0, 0), (0, 1)])
              S.barrier()
              done1 = set()

              def p1(ui):
                  if ui < len(units) and ui not in done1:
                      done1.add(ui)
                      part1(*units[ui])

              p1(0)
              for ui, (c, t) in enumerate(units):
                  p1(ui + 1)
                  if c > 0 and t == 0:
                      stage_sq(c - 1, 0)
                  if c > 0 and t == 2:
                      stage_sq(c - 1, 1)
                      stage_ssq(c - 1)
                  part2(c, t)
                  if c > 0 and t == 0:
                      stage_c(c - 1, 1)
                  if c == 3 and t == 1:
                      S.dma("gpsimd", "stg0", stg[0][:, 0:4096].rearrange("p (k c) -> p k c", k=8), win_v[:, :, 1024:1536],
                            writes=[("stg", 0), ("stgz", 0)] + [("szl", 0, t_) for t_ in range(4)])
                  if t == 3:
                      stage_b(c)
                      p1(ui + 2)
                      stage_c(c, 0)
                      if c == 3:
                          stage_c(c, 1)
                          stage_sq(c, 0)
                          stage_sq(c, 1)
                          v_proj()
                      if c + 2 < 4:
                          lru_load(c + 2)
              stage_ssq(3)
              dump("cclru%d" % l, cc[:, 4:8, :], [128, 4, S_LEN], BF16,
                   [("cc", 4 + c, g) for c in range(4) for g in range(4)])
              dump("ssql%d" % l, ssql, [128, 16], F32, [("ssql", 0), ("ssql", 1)])
              S.barrier()
              if "stopP1" in dbg:
                  break

              qk = {("q", 0): U[:, 0:2048], ("k", 0): U[:, 2048:4096],
                    ("q", 1): U[:, 4096:6144], ("k", 1): U[:, 6144:8192]}
              szT = U[:, 8192:10240]
              gp = U[:, 10240:12288].rearrange("p (t f) -> p t f", t=16)
              PT = [mod_bc.bitcast(BF16)[:, i * 512:(i + 1) * 512] for i in range(4)]
              mt = U[:, 13312:13888].rearrange("p (j c) -> p j c", j=8)
              g8 = Uf[:, 6944:7008]
              top8 = Uf[:, 7008:7072]
              ltm = Uf[:, 7072:7136]
              kmf = Uf[:, 7136:7144]
              kmb = U[:, 14288:14296]
              junk64f = Uf[:, 11328:11392]
              ssqp = smc("ssqp", 128)
              rden = smc("rden8", 8)
              S.op("vector", lambda e: e.memset(qk[("q", 1)][0:64, :], 0.0), writes=[("qk", "q", 1, g) for g in range(4)])
              S.op("vector", lambda e: e.memset(qk[("k", 1)][0:64, :], 0.0), writes=[("qk", "k", 1, g) for g in range(4)])
              S.op("vector", lambda e: e.memset(qk[("q", 0)][64:72, :], 0.0), writes=[("qk", "q", 0, g) for g in range(4)])
              S.op("vector", lambda e: e.memset(U[:, 13312:13888], 0.0), writes=[("mt",)])
              S.dma("sync", "kr0", qk[("k", 0)][64:73, :], krows_d, writes=[("qk", "k", 0, g) for g in range(4)])
              S.dma("sync", "kr1", qk[("k", 1)][0:9, :], krows_d, writes=[("qk", "k", 1, g) for g in range(4)])
              S.dma("sync", "qo0", qk[("q", 0)][73:74, :], krows_d[8:9, :], writes=[("qk", "q", 0, g) for g in range(4)])
              S.dma("sync", "qo1", qk[("q", 1)][9:10, :], krows_d[8:9, :], writes=[("qk", "q", 1, g) for g in range(4)])
              S.dma("sync", "qo2", qk[("q", 0)][75:76, :], krows_d[8:9, :], writes=[("qk", "q", 0, g) for g in range(4)])
              S.dma("sync", "qo3", qk[("q", 1)][11:12, :], krows_d[8:9, :], writes=[("qk", "q", 1, g) for g in range(4)])
              S.dma("sync", "ko2", qk[("k", 0)][74:75, :], krows_d[8:9, :], writes=[("qk", "k", 0, g) for g in range(4)])
              S.dma("sync", "ko3", qk[("k", 1)][10:11, :], krows_d[8:9, :], writes=[("qk", "k", 1, g) for g in range(4)])
              wo_t = [stg[0][:, 0:4096].rearrange("p (c d) -> p c d", c=4), stg[1][:, 0:4096].rearrange("p (c d) -> p c d", c=4)]

              def wo_ap(ch, lo=0, hi=D):
                  return wo_t[ch // 4][:, ch % 4, lo:hi]

              def wo_load(c0, c1, extra_keys):
                  for ch in range(c0, c1):
                      S.dma("gpsimd", "wo%d" % (ch % 2), wo_ap(ch), wout_d[l, ch * 128:(ch + 1) * 128, :],
                            writes=[("wo", ch)] + extra_keys)

              def wo_scale(c0, c1):
                  for ch in range(c0, c1):
                      S.op("vector", lambda e, ch=ch: e.tensor_tensor(
                          out=wo_ap(ch), in0=wo_ap(ch), in1=mod_bc[:, 2048:3072], op=ALU.mult),
                          reads=[("wo", ch), ("mod", 4), ("mod", 5)], writes=[("wo", ch)])

              for p in range(4):
                  wp = stg[1][:, 0:3072].rearrange("p (k c) -> p k c", k=8)
                  S.dma("gpsimd", "stg1", wp[:, :, 0:128], win_v[:, :, p * 128:(p + 1) * 128], writes=[("stgp", 0)])
                  S.dma("gpsimd", "stg1b", wp[:, :, 128:256], win_v[:, :, 512 + p * 128:512 + (p + 1) * 128], writes=[("stgp", 1)])
                  S.dma("gpsimd", "stg1c", wp[:, :, 256:384], win_v[:, :, 1536 + p * 128:1536 + (p + 1) * 128], writes=[("stgp", 2)])
                  S.dma("sync", "ar0", qk[("q", 0)][72:73, :], arow_d[2 * p:2 * p + 1, :], writes=[("qk", "q", 0, g) for g in range(4)])
                  S.dma("sync", "ar1", qk[("q", 1)][8:9, :], arow_d[2 * p + 1:2 * p + 2, :], writes=[("qk", "q", 1, g) for g in range(4)])
                  S.dma("sync", "ka0", qk[("k", 0)][73:74, :], karow_d[2 * p:2 * p + 1, :], writes=[("qk", "k", 0, g) for g in range(4)])
                  S.dma("sync", "ka1", qk[("k", 1)][9:10, :], karow_d[2 * p + 1:2 * p + 2, :], writes=[("qk", "k", 1, g) for g in range(4)])
                  S.dma("sync", "ah0", qk[("q", 0)][74:75, :], arowhi_d[2 * p:2 * p + 1, :], writes=[("qk", "q", 0, g) for g in range(4)])
                  S.dma("sync", "ah1", qk[("q", 1)][10:11, :], arowhi_d[2 * p + 1:2 * p + 2, :], writes=[("qk", "q", 1, g) for g in range(4)])
                  S.dma("sync", "kh0", qk[("k", 0)][75:76, :], karowhi_d[2 * p:2 * p + 1, :], writes=[("qk", "k", 0, g) for g in range(4)])
                  S.dma("sync", "kh1", qk[("k", 1)][11:12, :], karowhi_d[2 * p + 1:2 * p + 2, :], writes=[("qk", "k", 1, g) for g in range(4)])
                  pi_ = [0]

                  PB = [0, 1, 4, 5]

                  def proj(which, wi, tg):
                      pq = ps[PB[pi_[0] % 4]]
                      pkey = ("ps", PB[pi_[0] % 4])
                      pi_[0] += 1
                      hkeys = [("hT", 4 * tg + j) for j in range(4)]
                      for k in range(8):
                          S.op("tensor", lambda e, k=k: e.matmul(
                              pq[:, :], lhsT=wp[:, k, wi * 128:(wi + 1) * 128], rhs=hT[:, k, tg * 512:(tg + 1) * 512],
                              start=(k == 0), stop=(k == 7)),
                              reads=hkeys + [("stgp", wi)], writes=[pkey])
                      if which in "qk":
                          S.op("vector", lambda e: e.tensor_copy(
                              out=qk[(which, 0)][0:64, tg * 512:(tg + 1) * 512], in_=pq[0:64, :]),
                              reads=[pkey], writes=[("qk", which, 0, tg)])
                          S.op("scalar", lambda e: e.activation(
                              out=qk[(which, 1)][64:128, tg * 512:(tg + 1) * 512], in_=pq[64:128, :], func=AF.Copy),
                              reads=[pkey], writes=[("qk", which, 1, tg)])
                      else:
                          S.op("scalar", lambda e: e.activation(
                              out=szT[:, tg * 512:(tg + 1) * 512], in_=pq[:, :], func=AF.Tanh, scale=0.5),
                              reads=[pkey], writes=[("szT", tg)])
                          S.op("vector", lambda e: e.scalar_tensor_tensor(
                              out=szT[:, tg * 512:(tg + 1) * 512], in0=szT[:, tg * 512:(tg + 1) * 512], scalar=1.0,
                              in1=pq[:, :], op0=ALU.add, op1=ALU.mult),
                              reads=[pkey, ("szT", tg)], writes=[("szT", tg)])

                  def kmean(hb):
                      r0 = 64 * hb
                      kh = qk[("k", hb)]
                      kkeys = [("qk", "k", hb, g) for g in range(4)]
                      S.op("vector", lambda e: e.tensor_reduce(
                          out=kmf[r0:r0 + 64, :], in_=kh[r0:r0 + 64, :].rearrange("p (n t) -> p n t", n=8), axis=AX.X, op=ALU.add),
                          reads=kkeys, writes=[("kmf", hb)])
                      S.op("vector", lambda e: e.tensor_scalar(
                          out=kmb[r0:r0 + 64, :], in0=kmf[r0:r0 + 64, :], scalar1=1.0 / 256, scalar2=None, op0=ALU.mult),
                          reads=[("kmf", hb)], writes=[("kmb", hb)])

                  def gates(hb):
                      r0 = 64 * hb
                      qh = qk[("q", hb)]
                      for j in range(8):
                          S.op("tensor", lambda e, j=j: e.matmul(
                              ps[2][:, j * 8:(j + 1) * 8], lhsT=qh[r0:r0 + 64, 1024 + j * 128:1024 + (j + 1) * 128],
                              rhs=kmb[r0:r0 + 64, :], start=True, stop=True),
                              reads=[("qk", "q", hb, 2 + j // 4), ("kmb", hb)], writes=[("ps", 2)])

                  def topk(hb):
                      S.op("vector", lambda e: e.tensor_tensor(out=g8, in0=ps[2][:, 0:64], in1=gmask[:, :], op=ALU.add),
                           reads=[("ps", 2), ("gmask",)], writes=[("g8",)])
                      for j in range(8):
                          S.op("vector", lambda e, j=j: e.max(out=top8[:, j * 8:(j + 1) * 8], in_=g8[:, j * 8:(j + 1) * 8]),
                               reads=[("g8",)], writes=[("top8", j)])
                      thr_b = bass.AP(top8.tensor, top8.offset + 2, [[top8.ap[0][0], 128], [8, 8], [0, 8]])
                      S.op("vector", lambda e: e.tensor_tensor(
                          out=ltm.rearrange("p (j n) -> p j n", j=8), in0=g8.rearrange("p (j n) -> p j n", j=8),
                          in1=thr_b, op=ALU.is_lt),
                          reads=[("g8",)] + [("top8", j) for j in range(8)], writes=[("ltm",)])
                      S.op("vector", lambda e: e.tensor_tensor(
                          out=mt[:, :, 64:72], in0=ltm.rearrange("p (j n) -> p j n", j=8),
                          in1=vneg[:, :].rearrange("p (j n) -> p j n", j=8), op=ALU.mult),
                          reads=[("ltm",), ("vneg",)], writes=[("mt",)])

                  def masks_T(hb):
                      qh = qk[("q", hb)]
                      a0 = 64 if hb == 0 else 0
                      for j in range(8):
                          if hb == 0:
                              S.op("tensor", lambda e, j=j: e.transpose(
                                  out=psb[3][0:72, j * 128:(j + 1) * 128], in_=mt[:, j, 0:72], identity=ident_bf[:, :]),
                                  reads=[("mt",), ("ident_bf",)], writes=[("ps", 3)])
                          else:
                              S.op("tensor", lambda e, j=j: e.transpose(
                                  out=psb[3][0:8, j * 128:(j + 1) * 128], in_=mt[:, j, 64:72], identity=ident_bf[:, :]),
                                  reads=[("mt",), ("ident_bf",)], writes=[("ps", 3)])
                      S.op("vector", lambda e: e.tensor_copy(
                          out=qh[a0:a0 + 8, 1024:2048], in_=psb[3][a0:a0 + 8, 0:1024]),
                          reads=[("ps", 3)], writes=[("qk", "q", hb, 2), ("qk", "q", hb, 3)])

                  if p == 1:
                      wo_load(0, 4, [("stg", 0)])
                  if p == 2:
                      wo_scale(0, 4)
                  for tg in range(4):
                      proj("k", 1, tg)
                  kmean(0)
                  kmean(1)
                  for tg in range(4):
                      proj("q", 0, tg)
                  gates(0)
                  topk(0)
                  proj("z", 2, 0)
                  proj("z", 2, 1)
                  masks_T(0)
                  gates(1)
                  topk(1)
                  proj("z", 2, 2)
                  proj("z", 2, 3)
                  masks_T(1)
                  if p == 3:
                      wo_load(4, 8, [("stgp", 0), ("stgp", 1), ("stgp", 2)])
                  stop_at("stopProj")
                  stop_at("stopMask")
                  for hb in range(2):
                      h = 2 * p + hb
                      r0 = 64 * hb
                      qh = qk[("q", hb)]
                      kh = qk[("k", hb)]
                      K1 = 76 if hb == 0 else 128
                      steps = [(G, kt) for G in range(4) for kt in range(4 * G + 4)]
                      LOOK = 3
                      SB = [4, 5, 0, 1]

                      def geom(i):
                          G, kt = steps[i]
                          rel = kt // 2 - 2 * G
                          if rel < 0:
                              return G, kt, 0, None
                          c0 = 256 * rel + 128 * (kt % 2)
                          return G, kt, c0, c0

                      def emit_S(i, qh=qh, kh=kh, K1=K1, hb=hb):
                          G, kt, c0, tri = geom(i)
                          st_, skey = ps[SB[i % 4]], ("ps", SB[i % 4])
                          S.op("tensor", lambda e: e.matmul(
                              st_[:, c0:512], lhsT=kh[0:K1, kt * 128:(kt + 1) * 128],
                              rhs=qh[0:K1, G * 512 + c0:(G + 1) * 512], start=True, stop=(tri is None)),
                              reads=[("qk", "k", hb, kt // 4), ("qk", "q", hb, G)], writes=[skey])
                          if tri is not None:
                              S.op("tensor", lambda e: e.matmul(
                                  st_[:, tri:tri + 128], lhsT=ident_bf[:, :], rhs=trimask[:, :], start=False, stop=True),
                                  reads=[("ident_bf",), ("trimask",)], writes=[skey])

                      def emit_E(i):
                          G, kt, c0, tri = geom(i)
                          st_, skey = ps[SB[i % 4]], ("ps", SB[i % 4])
                          S.op("scalar", lambda e: e.activation(
                              out=PT[i % 4][:, c0:512], in_=st_[:, c0:512], func=AF.Exp, scale=0.125),
                              reads=[skey], writes=[("PT", i % 4)])

                      def emit_PV(i, h=h, hb=hb):
                          G, kt, c0, tri = geom(i)
                          accb = ps[6 + G % 2]
                          akey = ("acc", G % 2)
                          for qt in range(c0 // 128, 4):
                              lastkt = 4 * G + qt
                              S.op("tensor", lambda e, qt=qt: e.matmul(
                                  accb[:, qt * 128:qt * 128 + 65], lhsT=PT[i % 4][:, qt * 128:(qt + 1) * 128],
                                  rhs=Vt[:, kt, h, :], start=(kt == 0 and qt == 0), stop=(kt == lastkt),
                                  skip_group_check=True),
                                  reads=[("PT", i % 4), ("V", kt), ("Vones",)], writes=[akey])
                          if kt == 4 * G + 3:
                              rd = rden[:, 4 * (G % 2):4 * (G % 2) + 4]
                              S.op("vector", lambda e: e.reciprocal(
                                  out=rd, in_=accb[:, 0:512].rearrange("p (t c) -> p t c", t=4)[:, :, 64]),
                                  reads=[akey], writes=[("rden", G % 2)])
                              for qt in range(4):
                                  tt = 4 * G + qt
                                  S.op("vector", lambda e, qt=qt, tt=tt: e.tensor_scalar(
                                      out=gp[:, tt, 64 * hb:64 * hb + 64], in0=accb[:, qt * 128:qt * 128 + 64],
                                      scalar1=rd[:, qt:qt + 1], scalar2=None, op0=ALU.mult),
                                      reads=[akey, ("rden", G % 2)], writes=[("gp", tt, hb)])
                                  S.op("vector", lambda e, qt=qt, tt=tt: e.scalar_tensor_tensor(
                                      out=junk64f, in0=accb[:, qt * 128:qt * 128 + 64], scalar=rd[:, qt:qt + 1],
                                      in1=gp[:, tt, 64 * hb:64 * hb + 64], op0=ALU.mult, op1=ALU.mult,
                                      accum_out=ssqp[:, h * 16 + tt:h * 16 + tt + 1]),
                                      reads=[akey, ("rden", G % 2), ("gp", tt, hb)], writes=[("junk64",), ("ssqp", h, tt)])

                      for i in range(len(steps) + LOOK):
                          if i < len(steps):
                              emit_S(i)
                          j = i - LOOK
                          if j >= 0:
                              emit_E(j)
                              emit_PV(j)
                  stop_at("stopAttnPair")
                  for tg in range(4):
                      tb = 3 if tg % 2 == 0 else 2
                      for j in range(4):
                          tt = 4 * tg + j
                          S.op("tensor", lambda e, tt=tt, j=j, tb=tb: e.transpose(
                              out=psb[tb][:, j * 128:(j + 1) * 128], in_=gp[:, tt, :], identity=ident_bf[:, :]),
                              reads=[("gp", tt, 0), ("gp", tt, 1), ("ident_bf",)], writes=[("ps", tb)])
                      S.op("vector", lambda e, tg=tg, p=p, tb=tb: e.scalar_tensor_tensor(
                          out=cc[:, p, tg * 512:(tg + 1) * 512], in0=psb[tb][:, 0:512], scalar=anwh[:, p:p + 1],
                          in1=szT[:, tg * 512:(tg + 1) * 512], op0=ALU.mult, op1=ALU.mult),
                          reads=[("ps", tb), ("szT", tg), ("halfcols", "anw")], writes=[("cc", p, tg)])
              ssqa = smc("ssqa", 16)
              S.op("vector", lambda e: e.tensor_reduce(
                  out=ssqa, in_=ssqp.rearrange("p (h t) -> p t h", h=8), axis=AX.X, op=ALU.add),
                  reads=[("ssqp", h_, t_) for h_ in range(8) for t_ in range(16)], writes=[("ssqa",)])
              dump("ccattn%d" % l, cc[:, 0:4, :], [128, 4, S_LEN], BF16,
                   [("cc", c, g) for c in range(4) for g in range(4)])
              dump("ssqa%d" % l, ssqa, [128, 16], F32, [("ssqa",)])
              S.barrier()
              if "stopP2" in dbg:
                  break

              fnw_bc = Uf[:, 4096:5120]
              outt = [Uf[:, 5120:6144], Uf[:, 6144:7168]]
              junk3 = U[:, 14336:15360]
              rstda = smc("rstda", 16)
              rstdl = smc("rstdl", 16)
              for (dst, src_, key) in ((rstda, ssqa, ("ssqa",)), (rstdl, ssql, None)):
                  rk = [key] if key else [("ssql", 0), ("ssql", 1)]
                  S.op("gpsimd", lambda e, dst=dst, src_=src_: e.tensor_scalar(
                      out=dst, in0=src_, scalar1=1.0 / 512, scalar2=EPS, op0=ALU.mult, op1=ALU.add),
                      reads=rk, writes=[("rstd", id(dst))])
                  S.op("gpsimd", lambda e, dst=dst: e.tensor_tensor(
                      out=dst, in0=dst, in1=bass.AP(mhalf.tensor, mhalf.offset, [[mhalf.ap[0][0], 128], [0, 16]]), op=ALU.pow),
                      reads=[("rstd", id(dst)), ("mhalf",)], writes=[("rstd", id(dst))])
              rstd_keys = [("rstd", id(rstda)), ("rstd", id(rstdl))]
              wo_scale(4, 8)
              if final and last_layer:
                  fsrc = bass.AP(fnw_d.tensor, 0, [[0, 128], [1, D]])
                  S.dma("sync", "fnw", fnw_bc, fsrc, writes=[("fnw",)])
                  ssqf = smc("ssqf", 16)
                  rstdf = smc("rstdf", 16)
              def final_out(t_):
                  ot = outt[t_ % 2]
                  S.op("vector", lambda e: e.scalar_tensor_tensor(
                      out=ot, in0=x_sb[:, t_, :], scalar=rstdf[:, t_:t_ + 1], in1=fnw_bc, op0=ALU.mult, op1=ALU.mult),
                      reads=[("x", t_), ("rstdf", t_), ("fnw",)], writes=[("outt", t_ % 2)])
                  tok = S.dma("sync", "xout%d" % (t_ % 2), out_d[t_ * 128:(t_ + 1) * 128, :], ot,
                              reads=[("outt", t_ % 2)])
                  S.wait_at_end("sync", tok)

              cckeys = lambda tt: [("cc", ch, tt // 4) for ch in range(8)]
              it = 0
              nxt = (not last_layer)
              if nxt:
                  mprep_n = mod_prep(16384, 17408)
              for tt in range(NT):
                  if nxt and tt in (0, 2, 6, 8, 10, 12):
                      mod_dma(layers[li + 1], (0, 2, 6, 8, 10, 12).index(tt), True)
                  if nxt and tt % 2 == 1 and tt >= 5 and (tt - 5) // 2 < 6:
                      mod_mm(layers[li + 1], (tt - 5) // 2, mprep_n, True)
                  for dh in range(2):
                      nset = 2 if nxt else 3
                      pa = ps[2 * (it % nset)]
                      pl = ps[2 * (it % nset) + 1]
                      ka = ("ps", 2 * (it % nset))
                      kl = ("ps", 2 * (it % nset) + 1)
                      it += 1
                      for ch in range(4):
                          S.op("tensor", lambda e, ch=ch, tt=tt, dh=dh, pa=pa: e.matmul(
                              pa[:, :], lhsT=cc[:, ch, tt * 128:(tt + 1) * 128], rhs=wo_ap(ch, dh * 512, (dh + 1) * 512),
                              start=(ch == 0), stop=(ch == 3)),
                              reads=[("cc", ch, tt // 4), ("wo", ch)], writes=[ka])
                      for ch in range(4, 8):
                          S.op("tensor", lambda e, ch=ch, tt=tt, dh=dh, pl=pl: e.matmul(
                              pl[:, :], lhsT=cc[:, ch, tt * 128:(tt + 1) * 128], rhs=wo_ap(ch, dh * 512, (dh + 1) * 512),
                              start=(ch == 4), stop=(ch == 7)),
                              reads=[("cc", ch, tt // 4), ("wo", ch)], writes=[kl])
                      xs = x_sb[:, tt, dh * 512:(dh + 1) * 512]
                      S.op("vector", lambda e, xs=xs, pa=pa, tt=tt: e.scalar_tensor_tensor(
                          out=xs, in0=pa[:, :], scalar=rstda[:, tt:tt + 1], in1=xs, op0=ALU.mult, op1=ALU.add),
                          reads=[ka, ("x", tt)] + rstd_keys, writes=[("x", tt)])
                      S.op("vector", lambda e, xs=xs, pl=pl, tt=tt: e.scalar_tensor_tensor(
                          out=xs, in0=pl[:, :], scalar=rstdl[:, tt:tt + 1], in1=xs, op0=ALU.mult, op1=ALU.add),
                          reads=[kl, ("x", tt)] + rstd_keys, writes=[("x", tt)])
                  if nxt:
                      x_stats(tt, junk3)
                  if final and last_layer:
                      S.op("scalar", lambda e, tt=tt: e.activation(
                          out=junk3, in_=x_sb[:, tt, :], func=AF.Square, accum_out=ssqf[:, tt:tt + 1]),
                          reads=[("x", tt)], writes=[("junk3",), ("ssqf", tt)])
                      S.op("gpsimd", lambda e, tt=tt: e.tensor_scalar(
                          out=rstdf[:, tt:tt + 1], in0=ssqf[:, tt:tt + 1], scalar1=1.0 / D, scalar2=EPS,
                          op0=ALU.mult, op1=ALU.add),
                          reads=[("ssqf", tt)], writes=[("rstdf", tt)])
                      S.op("gpsimd", lambda e, tt=tt: e.tensor_tensor(
                          out=rstdf[:, tt:tt + 1], in0=rstdf[:, tt:tt + 1], in1=mhalf, op=ALU.pow),
                          reads=[("rstdf", tt), ("mhalf",)], writes=[("rstdf", tt)])
                      if tt >= 2:
                          final_out(tt - 2)
              if final and last_layer:
                  final_out(NT - 2)
                  final_out(NT - 1)
              elif last_layer:
                  for tt in range(NT):
                      tok = S.dma("sync", "xout%d" % (tt % 4), out_d[tt * 128:(tt + 1) * 128, :], x_sb[:, tt, :],
                                  reads=[("x", tt)])
                      S.wait_at_end("sync", tok)
              dump("xend%d" % l, x_sb[:, :, :], [128, NT, D], F32, [("x", t) for t in range(NT)])
              S.barrier()


        except _Stop:
            pass

        for sname, n in S.dma_cnt.items():
            S.wait_at_end("sync", ("d_" + sname, 16 * n))
        run = S.emit_all(None)
        with nc.Block() as block:
            @block.sync
            def _(e):
                run("sync", e)

            @block.scalar
            def _(e):
                run("scalar", e)

            @block.vector
            def _(e):
                run("vector", e)

            @block.gpsimd
            def _(e):
                run("gpsimd", e)

            @block.tensor
            def _(e):
                run("tensor", e)
    return nc, list(dbg_d.keys())


def make_inmaps(inp, L_total, x_override=None):
    consts = host_consts()
    inp = {k: np.asarray(v) for k, v in inp.items()}
    rab = blockdiag(inp["rg_a_w"].astype(np.float32))
    rxb = blockdiag(inp["rg_x_w"].astype(np.float32))
    xs = inp["x"] if x_override is None else x_override
    maps = []
    for b in range(8):
        m = {
            "x": np.ascontiguousarray(xs[b], dtype=np.float32),
            "cols": pack_cols(inp, b, L_total),
            "ada_w": np.ascontiguousarray(inp["ada_w"], dtype=np.float32),
            "ada_b": np.ascontiguousarray(inp["ada_b"], dtype=np.float32),
            "w_in": np.ascontiguousarray(inp["w_in"], dtype=np.float32),
            "w_out": np.ascontiguousarray(inp["w_out"], dtype=np.float32),
            "rg_a_bd": rab,
            "rg_x_bd": rxb,
            "final_norm_w": np.ascontiguousarray(inp["final_norm_w"], dtype=np.float32),
            "conv_b": np.ascontiguousarray(inp["conv_b"], dtype=np.float32),
        }
        m.update(consts)
        maps.append(m)
    return maps


_CACHE = {}


def kernel(**inputs):
    L_total = 2
    maps = make_inmaps(inputs, L_total)
    key = ("full",)
    if key not in _CACHE:
        _CACHE[key] = build([0, 1], True, L_total)[0]
    nc = _CACHE[key]
    res = run_bass_kernel_spmd(nc, maps, core_ids=list(range(8)))
    out = np.stack([np.asarray(r["out"], dtype=np.float32) for r in res.results], axis=0)
    return out
```

```python
import contextlib
import numpy as np
import ml_dtypes
import concourse.bass as bass
import concourse.mybir as mybir
from concourse.bass_utils import run_bass_kernel_spmd

F32 = mybir.dt.float32
BF16 = mybir.dt.bfloat16
AF = mybir.ActivationFunctionType
ALU = mybir.AluOpType
AX = mybir.AxisListType

S_LEN = 2048
D = 1024
NT = 16
H = 8
EPS = 1e-6
NEG = -30000.0
ENGS = ["sync", "scalar", "vector", "gpsimd", "tensor"]


class _Stop(Exception):
    pass


class Sched:
    def __init__(self, nc, stack):
        self.nc = nc
        self.stack = stack
        self.prog = {e: [] for e in ENGS}
        self.cnt = {e: 0 for e in ENGS}
        self.sems = {}
        self.seen = {e: {} for e in ENGS}
        self.last_w = {}
        self.readers = {}
        self.dma_cnt = {}
        self.pending = {e: {} for e in ENGS}
        self.waited = set()
        self.final = {e: {} for e in ENGS}

    def sem(self, name):
        if name not in self.sems:
            self.sems[name] = self.stack.enter_context(self.nc.semaphore(name))
        return self.sems[name]

    def _collect(self, eng, reads, writes, extra=()):
        waits = dict(self.pending[eng])
        self.pending[eng] = {}

        def need(tok):
            if tok is None:
                return
            s, v = tok
            if v > waits.get(s, 0):
                waits[s] = v

        for k in reads:
            need(self.last_w.get(k))
        for k in writes:
            need(self.last_w.get(k))
            for s, v in self.readers.get(k, {}).items():
                need((s, v))
        for t in extra:
            need(t)
        wl = []
        for s, v in waits.items():
            if eng == "tensor" and s == "e_tensor":
                continue
            if self.seen[eng].get(s, 0) >= v:
                continue
            self.seen[eng][s] = v
            self.waited.add((s, v))
            wl.append((s, v))
        return wl

    def _commit(self, tok, reads, writes):
        for k in writes:
            self.last_w[k] = tok
            self.readers[k] = {}
        for k in reads:
            r = self.readers.setdefault(k, {})
            if tok[1] > r.get(tok[0], 0):
                r[tok[0]] = tok[1]

    def op(self, eng, fn, reads=(), writes=()):
        wl = self._collect(eng, reads, writes)
        self.cnt[eng] += 1
        tok = ("e_" + eng, self.cnt[eng])
        self.sem(tok[0])
        self.prog[eng].append((wl, fn, tok, False))
        self._commit(tok, reads, writes)
        return tok

    def dma(self, eng, stream, out, in_, reads=(), writes=(), **kw):
        n = self.dma_cnt.get(stream, 0)
        extra = [("d_" + stream, 16 * n)] if n > 0 else []
        wl = self._collect(eng, reads, writes, extra)
        self.dma_cnt[stream] = n + 1
        tok = ("d_" + stream, 16 * (n + 1))
        self.sem(tok[0])
        self.prog[eng].append((wl, (lambda e: e.dma_start(out=out, in_=in_, **kw)), tok, True))
        self._commit(tok, reads, writes)
        return tok

    def barrier(self):
        toks = {}
        for e in ENGS:
            if self.cnt[e] > 0:
                toks["e_" + e] = self.cnt[e]
        for s, n in self.dma_cnt.items():
            toks["d_" + s] = 16 * n
        for e in ENGS:
            for s, v in toks.items():
                if v > self.pending[e].get(s, 0):
                    self.pending[e][s] = v
        self.last_w = {}
        self.readers = {}

    def wait_at_end(self, eng, tok):
        self.final[eng][tok[0]] = max(self.final[eng].get(tok[0], 0), tok[1])
        self.waited.add(tok)

    def emit(self, eng, e):
        remap = {}
        run = 0
        for (wl, fn, tok, is_dma) in self.prog[eng]:
            if not is_dma and tok in self.waited:
                run += 1
                remap[tok[1]] = run
        self._remaps[eng] = remap

    def emit_all(self, block_engines):
        self._remaps = {}
        for eng in ENGS:
            self.emit(eng, None)

        def mapped(s, v):
            if s.startswith("e_"):
                return self._remaps[s[2:]][v]
            return v

        def run(eng, e):
            for (wl, fn, tok, is_dma) in self.prog[eng]:
                for (s, v) in wl:
                    e.wait_ge(self.sems[s], mapped(s, v))
                inst = fn(e)
                if is_dma:
                    inst.then_inc(self.sems[tok[0]], 16)
                elif tok in self.waited:
                    inst.then_inc(self.sems[tok[0]], 1)
            for s, v in self.pending[eng].items():
                pass
            for s, v in self.final[eng].items():
                e.wait_ge(self.sems[s], mapped(s, v))

        return run


def alibi_slopes():
    return [2.0 ** (-8.0 * (i + 1) / H) for i in range(H)]


def host_consts():
    sl = alibi_slopes()
    c = {}
    c["ident_bf"] = np.eye(128, dtype=np.float32).astype(ml_dtypes.bfloat16)
    c["ident_f"] = np.eye(128, dtype=np.float32)
    k = np.arange(128)[:, None]
    q = np.arange(128)[None, :]
    c["trimask"] = np.where(q >= k, 0.0, NEG).astype(np.float32).astype(ml_dtypes.bfloat16)
    bt = np.zeros((128, H, 17), np.float32)
    for h in range(H):
        for j in range(17):
            bt[:, h, j] = sl[h] * (np.arange(128) - 128.0 * (j - 1))
    c["bias_tab"] = bt.reshape(128, H * 17)
    qi = (np.arange(S_LEN) % 256).astype(np.float32)
    c["arow"] = np.stack([-8.0 * sl[h] * qi for h in range(H)]).astype(np.float32).astype(ml_dtypes.bfloat16)
    c["karow"] = np.stack([8.0 * sl[h] * qi for h in range(H)]).astype(np.float32).astype(ml_dtypes.bfloat16)
    qbi = (np.arange(S_LEN) // 256).astype(np.float32)
    c["arow_hi"] = np.stack([-8.0 * sl[h] * 256.0 * qbi for h in range(H)]).astype(np.float32).astype(ml_dtypes.bfloat16)
    c["karow_hi"] = np.stack([8.0 * sl[h] * 256.0 * qbi for h in range(H)]).astype(np.float32).astype(ml_dtypes.bfloat16)
    kr = np.zeros((9, S_LEN), np.float32)
    for n in range(8):
        kr[n, n * 256:(n + 1) * 256] = 1.0
    kr[8, :] = 1.0
    c["krows"] = kr.astype(ml_dtypes.bfloat16)
    gm = np.zeros((128, 8, 8), np.float32)
    for j in range(8):
        qb = 4 + j // 2
        gm[:, j, qb:] = -1e30
    c["gmask"] = gm.reshape(128, 64)
    vn = np.zeros((128, 8, 8), np.float32)
    for j in range(8):
        qb = 4 + j // 2
        vn[:, j, :qb] = NEG
    c["vneg"] = vn.reshape(128, 64)
    c["ones_bf"] = np.ones((128, 128), np.float32).astype(ml_dtypes.bfloat16)
    return c


def col_layout(L):
    off = {}
    n = 0

    def add(name, w):
        nonlocal n
        off[name] = (n, w)
        n += w

    add("cT", 8)
    for l in range(L):
        add(("normw", l), 8)
        add(("convw", l), 16)
        add(("convb", l), 4)
        add(("rab", l), 4)
        add(("rxb", l), 4)
        add(("lam", l), 4)
        add(("anw", l), 4)
        add(("lnw", l), 4)
    return off, n


def pack_cols(inp, b, L):
    off, n = col_layout(L)
    cols = np.zeros((128, n), np.float32)

    def put(name, arr):
        o, w = off[name]
        assert arr.shape == (128, w), (name, arr.shape)
        cols[:, o:o + w] = arr

    put("cT", inp["c"][b].reshape(8, 128).T)
    for l in range(L):
        put(("normw", l), inp["norm_w"][l].reshape(8, 128).T)
        cw = inp["conv_w"][l]
        put(("convw", l), cw.reshape(4, 4, 128).transpose(2, 1, 0).reshape(128, 16))
        for nm, key in (("convb", "conv_b"), ("rab", "rg_a_b"), ("rxb", "rg_x_b"), ("lam", "rg_lambda"),
                        ("anw", "attn_out_norm_w"), ("lnw", "lru_out_norm_w")):
            put((nm, l), inp[key][l].reshape(4, 128).T)
    return cols


def blockdiag(w):
    L = w.shape[0]
    out = np.zeros((L, 4, 128, 128), np.float32)
    for c in range(4):
        out[:, c, 0:64, 0:64] = w[:, 2 * c]
        out[:, c, 64:128, 64:128] = w[:, 2 * c + 1]
    return out


def build(layers, final, L_total, dbg=()):
    nc = bass.Bass("TRN2", target_bir_lowering=False)
    coff, ncol = col_layout(L_total)
    dr = {}

    def din(name, shape, dt=F32):
        dr[name] = nc.dram_tensor(name, list(shape), dt, kind="ExternalInput").ap()
        return dr[name]

    x_d = din("x", [S_LEN, D])
    cols_d = din("cols", [128, ncol])
    adaw_d = din("ada_w", [L_total, D, 3 * D])
    adab_d = din("ada_b", [L_total, 3 * D])
    win_d = din("w_in", [L_total, D, 3 * D])
    wout_d = din("w_out", [L_total, D, D])
    raw_d = din("rg_a_bd", [L_total, 4, 128, 128])
    rxw_d = din("rg_x_bd", [L_total, 4, 128, 128])
    fnw_d = din("final_norm_w", [D])
    din("conv_b", [L_total, 512])
    identbf_d = din("ident_bf", [128, 128], BF16)
    identf_d = din("ident_f", [128, 128])
    trimask_d = din("trimask", [128, 128], BF16)
    biastab_d = din("bias_tab", [128, H * 17])
    arow_d = din("arow", [H, S_LEN], BF16)
    krows_d = din("krows", [9, S_LEN], BF16)
    karow_d = din("karow", [H, S_LEN], BF16)
    arowhi_d = din("arow_hi", [H, S_LEN], BF16)
    karowhi_d = din("karow_hi", [H, S_LEN], BF16)
    gmask_d = din("gmask", [128, 64])
    vneg_d = din("vneg", [128, 64])
    onesbf_d = din("ones_bf", [128, 128], BF16)
    out_d = nc.dram_tensor("out", [S_LEN, D], F32, kind="ExternalOutput").ap()
    dbg_d = {}

    UW = 22784
    with contextlib.ExitStack() as st:
        def sb(name, shape, dt):
            return st.enter_context(nc.sbuf_tensor(name, list(shape), dt))

        x_sb = sb("x_sb", [128, NT, D], F32)
        hT = sb("hT", [128, 8, S_LEN], BF16)
        cc = sb("cc", [128, 8, S_LEN], BF16)
        stg = [sb("stg0", [128, 4096], BF16), sb("stg1", [128, 4096], BF16)]
        mod_bc = sb("mod_bc", [128, 3 * D], F32)
        U = sb("U", [128, UW], BF16)
        Uf = U.bitcast(F32)
        ident_bf = sb("ident_bf_s", [128, 128], BF16)
        ident_f = sb("ident_f_s", [128, 128], F32)
        trimask = sb("trimask_s", [128, 128], BF16)
        bias_tab = sb("bias_tab_s", [128, H * 17], F32)
        gmask = sb("gmask_s", [128, 64], F32)
        vneg = sb("vneg_s", [128, 64], F32)
        ones_bf = sb("ones_bf_s", [128, 128], BF16)
        ones4 = sb("ones4_s", [1, 512], BF16)
        cols = sb("cols_s", [128, ncol], F32)
        sm = sb("small", [128, 512], F32)
        ps = [st.enter_context(nc.psum_tensor("ps%d" % i, [128, 512], F32)) for i in range(8)]
        psb = [p.bitcast(BF16) for p in ps]

        S = Sched(nc, st)

        def col(name, j0=0, w=None):
            o, ww = coff[name]
            if w is None:
                w = ww - j0
            return cols[:, o + j0:o + j0 + w]

        smo = {}
        smn = [0]

        def smc(name, w):
            if name not in smo:
                smo[name] = (smn[0], w)
                smn[0] += w
                assert smn[0] <= 512
            o, ww = smo[name]
            return sm[:, o:o + ww]

        onesrow = ones4[0:1, :]
        S.dma("sync", "c8", onesrow, krows_d[8:9, 0:512], writes=[("onesrow",)])
        qcol = smc("qcol", 1)
        mhalf = smc("mhalf", 1)
        S.op("vector", lambda e: e.memset(mhalf, -0.5), writes=[("mhalf",)])
        S.op("vector", lambda e: e.memset(qcol, 0.25), writes=[("qcol",)])
        S.dma("sync", "c0", cols[:, :], cols_d, writes=[("cols",)])
        S.dma("sync", "c1", ident_bf[:, :], identbf_d, writes=[("ident_bf",)])
        S.dma("sync", "c2", ident_f[:, :], identf_d, writes=[("ident_f",)])
        S.dma("sync", "c3", trimask[:, :], trimask_d, writes=[("trimask",)])
        S.dma("sync", "c4", bias_tab[:, :], biastab_d, writes=[("bias_tab",)])
        S.dma("sync", "c5", gmask[:, :], gmask_d, writes=[("gmask",)])
        S.dma("sync", "c7", vneg[:, :], vneg_d, writes=[("vneg",)])
        S.dma("sync", "c6", ones_bf[:, :], onesbf_d, writes=[("ones_bf",)])

        dbg_cnt = [0]

        def dump(name, ap, shape, dt, reads):
            if name not in dbg:
                return
            t = nc.dram_tensor("dbg_" + name, list(shape), dt, kind="ExternalOutput").ap()
            dbg_d[name] = t
            tok = S.dma("sync", "dbg%d" % dbg_cnt[0], t, ap, reads=reads)
            dbg_cnt[0] += 1
            S.wait_at_end("sync", tok)


        dparts = {}

        def dump_part(name, c, hf, ap, reads):
            if name not in dparts:
                dparts[name] = nc.dram_tensor("dbg_" + name, [128, 4, S_LEN], F32, kind="ExternalOutput").ap()
                dbg_d[name] = dparts[name]
            tok = S.dma("sync", "dbg%d" % dbg_cnt[0], dparts[name][:, c, hf * 1024:(hf + 1) * 1024], ap, reads=reads)
            dbg_cnt[0] += 1
            S.wait_at_end("sync", tok)

        ssqx = smc("ssqx", 16)
        rstdx = smc("rstdx", 16)

        def x_stats(tt, junk_ap):
            S.op("scalar", lambda e: e.activation(
                out=junk_ap, in_=x_sb[:, tt, :], func=AF.Square, accum_out=ssqx[:, tt:tt + 1]),
                reads=[("x", tt)], writes=[("junkx",), ("ssqx", tt)])
            S.op("gpsimd", lambda e: e.tensor_scalar(
                out=rstdx[:, tt:tt + 1], in0=ssqx[:, tt:tt + 1], scalar1=1.0 / D, scalar2=EPS, op0=ALU.mult, op1=ALU.add),
                reads=[("ssqx", tt)], writes=[("rstdx", tt)])
            S.op("gpsimd", lambda e: e.tensor_tensor(
                out=rstdx[:, tt:tt + 1], in0=rstdx[:, tt:tt + 1], in1=mhalf, op=ALU.pow),
                reads=[("rstdx", tt), ("mhalf",)], writes=[("rstdx", tt)])

        def mod_prep(scoff, aboff):
            th = smc("th", 8)
            sc = smc("sc", 8)
            cT = col("cT")
            S.op("scalar", lambda e: e.activation(out=th, in_=cT, func=AF.Tanh, scale=0.5),
                 reads=[("cols",)], writes=[("th",)])
            S.op("vector", lambda e: e.scalar_tensor_tensor(
                out=sc, in0=th, scalar=1.0, in1=cT, op0=ALU.add, op1=ALU.mult),
                reads=[("th",), ("cols",)], writes=[("sc",)])
            S.op("vector", lambda e: e.tensor_scalar(out=sc, in0=sc, scalar1=0.5, scalar2=None, op0=ALU.mult),
                 reads=[("sc",)], writes=[("sc",)])
            screp = U[:, scoff:scoff + 1024].rearrange("p (k m) -> p k m", k=8)
            for k in range(8):
                S.op("vector", lambda e, k=k: e.tensor_scalar(
                    out=screp[:, k, :], in0=ones_bf[:, :], scalar1=sc[:, k:k + 1], scalar2=None, op0=ALU.mult),
                    reads=[("sc",), ("ones_bf",)], writes=[("screp", k)])
            return screp, aboff

        def mod_bufs(in_u):
            if in_u:
                return [U[:, 0:4096], U[:, 4096:8192]], "ustg"
            return [stg[0][:, 0:4096], stg[1][:, 0:4096]], "stg"

        def mod_dma(l_, ng, in_u=False):
            adaw_v = adaw_d[l_].rearrange("(k p) c -> p k c", p=128)
            bufs, kn = mod_bufs(in_u)
            sgv = bufs[ng % 2].rearrange("p (k c) -> p k c", k=8)
            S.dma("gpsimd", "%s%d" % (kn, ng % 2), sgv, adaw_v[:, :, ng * 512:(ng + 1) * 512],
                  writes=[(kn, ng % 2), ("adaload", ng)])

        def mod_mm(l_, ng, mprep, in_u=False):
            screp, aboff = mprep
            bufs, kn = mod_bufs(in_u)
            if ng == 0:
                S.dma("gpsimd", "adab", U[0:1, aboff:aboff + 3 * D], adab_d[l_:l_ + 1, :], writes=[("adab",)],
                      max_dma_last_dim=2048)
            sgv = bufs[ng % 2].rearrange("p (k c) -> p k c", k=8)
            pt = ps[4 + ng % 2]
            pk = ("ps", 4 + ng % 2)
            for k in range(8):
                S.op("tensor", lambda e, k=k: e.matmul(
                    pt[:, :], lhsT=screp[:, k, :], rhs=sgv[:, k, :], start=(k == 0), stop=False),
                    reads=[("screp", k), (kn, ng % 2)], writes=[pk])
            S.op("tensor", lambda e: e.matmul(
                pt[:, :], lhsT=ones_bf[0:1, :], rhs=U[0:1, aboff + ng * 512:aboff + (ng + 1) * 512], start=False, stop=True),
                reads=[("ones_bf",), ("adab",)], writes=[pk])
            S.op("scalar", lambda e: e.activation(
                out=mod_bc[:, ng * 512:(ng + 1) * 512], in_=pt[:, :], func=AF.Copy),
                reads=[pk], writes=[("mod", ng)])

        first = True

        def stop_at(name):
            if name in dbg:
                raise _Stop()

        try:
          for li, l in enumerate(layers):
              last_layer = (li == len(layers) - 1)
              if li == 0:
                  mprep = mod_prep(0, 9216)
                  for ng in range(4):
                      mod_dma(l, ng)
                      mod_mm(l, ng, mprep)
                  mod_dma(l, 4)
                  mod_dma(l, 5)
              tmpd = Uf[:, 1024:1024 + 2048].rearrange("p (j m) -> p j m", j=16)
              identb = bass.AP(ident_f, 0, [[128, 128], [0, 16], [1, 128]])
              S.op("vector", lambda e: e.tensor_tensor(
                  out=tmpd, in0=mod_bc[:, 0:2048].rearrange("p (j m) -> p j m", j=16), in1=identb, op=ALU.mult),
                  reads=[("mod", g) for g in range(4)] + [("ident_f",)], writes=[("tmpd",)])
              col16 = smc("col16", 16)
              S.op("vector", lambda e: e.tensor_reduce(out=col16, in_=tmpd, axis=AX.X, op=ALU.add),
                   reads=[("tmpd",)], writes=[("col16",)])
              s1 = smc("s1", 8)
              S.op("vector", lambda e, l=l: e.scalar_tensor_tensor(
                  out=s1, in0=col16[:, 8:16], scalar=1.0, in1=col(("normw", l)), op0=ALU.add, op1=ALU.mult),
                  reads=[("col16",), ("cols",)], writes=[("s1",)])
              dump("mod%d" % l, mod_bc[:, :], [128, 3 * D], F32, [("mod", g) for g in range(6)])
              dump("s1_%d" % l, s1, [128, 8], F32, [("s1",)])

              junk = U[:, 6144:6144 + 1024]
              if li == 0:
                  for tt in range(NT):
                      if first:
                          S.dma("sync", "xin%d" % (tt % 4), x_sb[:, tt, :], x_d[tt * 128:(tt + 1) * 128, :],
                                reads=[("adaload", 2)], writes=[("x", tt)])
                      x_stats(tt, junk)
              xn = [U[:, 7168:7168 + 1024], U[:, 8192:8192 + 1024]]
              tmpf = [Uf[:, 6144:7168], Uf[:, 7168:8192]]
              s1_b = bass.AP(s1.tensor, s1.offset, [[s1.ap[0][0], 128], [1, 8], [0, 128]])
              sh_b = bass.AP(col16.tensor, col16.offset, [[col16.ap[0][0], 128], [1, 8], [0, 128]])
              shbf = sm.bitcast(BF16)[:, 960:968]
              S.op("vector", lambda e: e.tensor_copy(out=shbf, in_=col16[:, 0:8]), reads=[("col16",)], writes=[("shbf",)])
              shb_b = bass.AP(shbf.tensor, shbf.offset, [[shbf.ap[0][0], 128], [1, 8], [0, 128]])
              for tt in range(NT):
                  xb = xn[tt % 2]
                  S.op("scalar", lambda e, tt=tt, xb=xb: e.activation(
                      out=xb, in_=x_sb[:, tt, :], func=AF.Copy, scale=rstdx[:, tt:tt + 1]),
                      reads=[("x", tt), ("rstdx", tt)], writes=[("xn", tt % 2)])
                  pb = psb[2 + tt % 2]
                  for k in range(8):
                      S.op("tensor", lambda e, k=k, xb=xb, pb=pb: e.transpose(
                          out=pb[:, k * 128:(k + 1) * 128], in_=xb[:, k * 128:(k + 1) * 128], identity=ident_bf[:, :]),
                          reads=[("xn", tt % 2), ("ident_bf",)], writes=[("ps", 2 + tt % 2)])
                  S.op("vector", lambda e, pb=pb, tt=tt: e.tensor_tensor(
                      out=hT[:, :, tt * 128:(tt + 1) * 128], in0=pb[:, 0:1024].rearrange("p (k m) -> p k m", k=8),
                      in1=s1_b, op=ALU.mult),
                      reads=[("ps", 2 + tt % 2), ("s1",)], writes=[("hT", tt)])
                  S.op("vector", lambda e, tt=tt: e.tensor_tensor(
                      out=hT[:, :, tt * 128:(tt + 1) * 128], in0=hT[:, :, tt * 128:(tt + 1) * 128], in1=shb_b, op=ALU.add),
                      reads=[("hT", tt), ("shbf",)], writes=[("hT", tt)])
                  if li == 0 and tt in (5, 10):
                      mod_mm(l, 4 if tt == 5 else 5, mprep)
              dump("hT%d" % l, hT[:, :, :], [128, 8, S_LEN], BF16, [("hT", t) for t in range(NT)])
              dump("xin%d" % l, x_sb[:, :, :], [128, NT, D], F32, [("x", t) for t in range(NT)])
              dump("c16_%d" % l, col16, [128, 16], F32, [("col16",)])
              first = False
              if "stopP0" in dbg:
                  S.barrier()
                  break
              TU = 512
              Abig = Uf[:, 0:2048]
              SVbig = Uf[:, 2048:4096]
              Mbig = Uf[:, 4096:6144]
              szls = [stg[0][:, 2048:4096], stg[1][:, 2048:4096]]
              hsq = mod_bc.bitcast(BF16)[:, 0:2048]
              xlb = [U[:, 14336:14336 + 516], U[:, 14852:14852 + 516]]
              xcbs = [U[:, 15368:15880], U[:, 15880:16392]]
              rts = [Uf[:, 8196:8708], Uf[:, 8708:9220]]
              itv = Uf[:, 9220:9732]
              dgs = [U[:, 19464:19976], U[:, 19976:20488]]
              smb = sm.bitcast(BF16)
              wabs = [smb[:, 700:828], smb[:, 828:956]]
              wxbs = [U[:, 20488:20616], U[:, 20616:20744]]
              cbrow = [U[0:1, 20744:20872], U[0:1, 20872:21000]]
              lam = col(("lam", l))
              e1 = smc("e1", 4)
              nsp = smc("nsp", 4)
              hnsp = smc("hnsp", 4)
              habh = smc("habh", 4)
              hxbh = smc("hxbh", 4)
              lnwh = smc("lnwh", 4)
              anwh = smc("anwh", 4)
              ssql = smc("ssql", 16)
              hcar = smc("hcar", 1)
              S.op("scalar", lambda e, lam=lam: e.activation(out=e1, in_=lam, func=AF.Exp, scale=-1.0),
                   reads=[("cols",)], writes=[("e1",)])
              S.op("scalar", lambda e: e.activation(out=e1, in_=e1, func=AF.Ln, bias=1.0),
                   reads=[("e1",)], writes=[("e1",)])
              S.op("vector", lambda e: e.tensor_scalar(out=nsp, in0=e1, scalar1=-8.0, scalar2=None, op0=ALU.mult),
                   reads=[("e1",)], writes=[("nsp",)])
              S.op("vector", lambda e: e.tensor_scalar(out=hnsp, in0=e1, scalar1=-4.0, scalar2=None, op0=ALU.mult),
                   reads=[("e1",)], writes=[("hnsp",)])
              for (dst, srcn) in ((habh, "rab"), (hxbh, "rxb"), (lnwh, "lnw"), (anwh, "anw")):
                  S.op("vector", lambda e, dst=dst, srcn=srcn, l=l: e.tensor_scalar(
                      out=dst, in0=col((srcn, l)), scalar1=0.5, scalar2=None, op0=ALU.mult),
                      reads=[("cols",)], writes=[("halfcols", srcn)])
              dump("nsp%d" % l, nsp, [128, 4], F32, [("nsp",)])
              win_v = win_d[l].rearrange("(k p) c -> p k c", p=128)
              convb_d2 = dr["conv_b"]

              def lru_load(c):
                  sg = stg[c % 2]
                  wl_ = sg[:, 0:2048].rearrange("p (k c) -> p k c", k=8)
                  S.dma("gpsimd", "stg%d" % (c % 2), wl_[:, :, 0:128], win_v[:, :, 2048 + c * 128:2048 + (c + 1) * 128],
                        writes=[("stg", c % 2)])
                  S.dma("gpsimd", "stgb%d" % (c % 2), wl_[:, :, 128:256], win_v[:, :, 2560 + c * 128:2560 + (c + 1) * 128],
                        writes=[("stgz", c % 2), ("stg", c % 2)])
                  S.dma("gpsimd", "wab%d" % (c % 2), wabs[c % 2], raw_d[l, c], writes=[("wab", c % 2)])
                  S.dma("gpsimd", "wxb%d" % (c % 2), wxbs[c % 2], rxw_d[l, c], writes=[("wxb", c % 2)])
                  S.dma("gpsimd", "cbr%d" % (c % 2), cbrow[c % 2], convb_d2[l:l + 1, c * 128:(c + 1) * 128],
                        writes=[("cbrow", c % 2)])
                  cw = col(("convw", l), c * 4, 4)
                  for j in range(4):
                      S.op("vector", lambda e, j=j, cw=cw, c=c: e.tensor_scalar(
                          out=dgs[c % 2][:, j * 128:(j + 1) * 128], in0=ident_bf[:, :], scalar1=cw[:, j:j + 1], scalar2=None,
                          op0=ALU.mult),
                          reads=[("ident_bf",), ("cols",)], writes=[("dg", c % 2)])

              premm = set()

              def part1_mm(c, t):
                  b = (4 * c + t) % 2
                  wl_ = stg[c % 2][:, 0:2048].rearrange("p (k c) -> p k c", k=8)
                  pxl, pzl = ps[b], ps[2 + b]
                  kxl, kzl = ("ps", b), ("ps", 2 + b)
                  hkeys = [("hT", 4 * t + j) for j in range(4)]
                  for k in range(8):
                      S.op("tensor", lambda e, k=k: e.matmul(
                          pxl[:, :], lhsT=wl_[:, k, 0:128], rhs=hT[:, k, t * TU:(t + 1) * TU], start=(k == 0), stop=(k == 7)),
                          reads=hkeys + [("stg", c % 2)], writes=[kxl])
                  for k in range(8):
                      S.op("tensor", lambda e, k=k: e.matmul(
                          pzl[:, :], lhsT=wl_[:, k, 128:256], rhs=hT[:, k, t * TU:(t + 1) * TU], start=(k == 0), stop=(k == 7)),
                          reads=hkeys + [("stgz", c % 2)], writes=[kzl])

              def part1(c, t):
                  u = 4 * c + t
                  b = u % 2
                  wl_ = stg[c % 2][:, 0:2048].rearrange("p (k c) -> p k c", k=8)
                  xl_ = xlb[b]
                  pxl, pzl, pcv = ps[b], ps[2 + b], ps[4 + b]
                  kxl, kzl, kcv = ("ps", b), ("ps", 2 + b), ("ps", 4 + b)
                  hkeys = [("hT", 4 * t + j) for j in range(4)]
                  if (c, t) not in premm:
                      part1_mm(c, t)
                  if t == 0:
                      S.op("vector", lambda e: e.memset(xl_[:, 0:3], 0.0), writes=[("xlb", b, "h")])
                  else:
                      S.op("vector", lambda e: e.tensor_copy(out=xl_[:, 0:3], in_=xlb[1 - b][:, TU:TU + 3]),
                           reads=[("xlb", 1 - b)], writes=[("xlb", b, "h")])
                  S.op("vector", lambda e: e.tensor_copy(out=xl_[:, 3:3 + TU], in_=pxl[:, :]),
                       reads=[kxl], writes=[("xlb", b)])
                  S.op("scalar", lambda e: e.activation(out=rts[b], in_=pzl[:, :], func=AF.Tanh, scale=0.5),
                       reads=[kzl], writes=[("rt", b)])
                  S.op("vector", lambda e: e.scalar_tensor_tensor(
                      out=szls[c % 2][:, t * TU:(t + 1) * TU], in0=rts[b], scalar=1.0, in1=pzl[:, :], op0=ALU.add, op1=ALU.mult),
                      reads=[("rt", b), kzl], writes=[("szl", c % 2, t)])
                  for j in range(4):
                      S.op("tensor", lambda e, j=j: e.matmul(
                          pcv[:, :], lhsT=dgs[c % 2][:, j * 128:(j + 1) * 128], rhs=xl_[:, j:j + TU], start=(j == 0), stop=False),
                          reads=[("dg", c % 2), ("xlb", b), ("xlb", b, "h")], writes=[kcv])
                  S.op("tensor", lambda e: e.matmul(
                      pcv[:, :], lhsT=cbrow[c % 2], rhs=ones_bf[0:1, 0:TU] if False else onesrow, start=False, stop=True),
                      reads=[("cbrow", c % 2), ("onesrow",)], writes=[kcv])
                  S.op("scalar", lambda e: e.activation(out=xcbs[b], in_=pcv[:, :], func=AF.Copy),
                       reads=[kcv], writes=[("xcb", b)])

              def part2(c, t):
                  u = 4 * c + t
                  b = u % 2
                  pcv, kcv = ps[4 + b], ("ps", 4 + b)
                  rt_ = rts[b]
                  S.op("tensor", lambda e: e.matmul(ps[6][:, :], lhsT=wabs[c % 2], rhs=xcbs[b], start=True, stop=True),
                       reads=[("wab", c % 2), ("xcb", b)], writes=[("ps", 6)])
                  S.op("tensor", lambda e: e.matmul(ps[7][:, :], lhsT=wxbs[c % 2], rhs=xcbs[b], start=True, stop=True),
                       reads=[("wxb", c % 2), ("xcb", b)], writes=[("ps", 7)])
                  S.op("scalar", lambda e: e.activation(
                      out=rt_, in_=ps[6][:, :], func=AF.Tanh, scale=0.5, bias=habh[:, c:c + 1]),
                      reads=[("ps", 6), ("halfcols", "rab"), ("szl", c % 2, t)], writes=[("rt", b)])
                  S.op("scalar", lambda e: e.activation(
                      out=itv, in_=ps[7][:, :], func=AF.Tanh, scale=0.5, bias=hxbh[:, c:c + 1]),
                      reads=[("ps", 7), ("halfcols", "rxb")], writes=[("itv",)])
                  S.op("scalar", lambda e: e.activation(
                      out=Abig[:, t * TU:(t + 1) * TU], in_=rt_, func=AF.Exp, scale=hnsp[:, c:c + 1], bias=hnsp[:, c:c + 1]),
                      reads=[("rt", b), ("hnsp",)], writes=[("A", t)])
                  S.op("scalar", lambda e: e.activation(
                      out=SVbig[:, t * TU:(t + 1) * TU], in_=rt_, func=AF.Exp, scale=nsp[:, c:c + 1], bias=nsp[:, c:c + 1]),
                      reads=[("rt", b), ("nsp",)], writes=[("SV", t)])
                  S.op("vector", lambda e: e.scalar_tensor_tensor(
                      out=Mbig[:, t * TU:(t + 1) * TU], in0=itv, scalar=1.0, in1=pcv[:, :], op0=ALU.add, op1=ALU.mult),
                      reads=[("itv",), kcv], writes=[("M", t)])

              allk = lambda nm: [(nm, t) for t in range(4)]

              def stage_b(c):
                  for hf in range(2):
                      sl_ = slice(hf * 1024, (hf + 1) * 1024)
                      S.op("scalar", lambda e, sl_=sl_: e.activation(
                          out=SVbig[:, sl_], in_=SVbig[:, sl_], func=AF.Sqrt, scale=-0.25, bias=qcol),
                          reads=[("SV", 2 * hf), ("SV", 2 * hf + 1), ("qcol",)], writes=[("SV", 2 * hf), ("SV", 2 * hf + 1)])

              def stage_c(c, hf):
                  sl_ = slice(hf * 1024, (hf + 1) * 1024)
                  ks = lambda nm: [(nm, 2 * hf), (nm, 2 * hf + 1)]
                  S.op("vector", lambda e: e.tensor_tensor(
                      out=Mbig[:, sl_], in0=SVbig[:, sl_], in1=Mbig[:, sl_], op=ALU.mult),
                      reads=ks("SV") + ks("M"), writes=ks("M"))
                  if hf == 0:
                      S.op("vector", lambda e: e.tensor_tensor_scan(
                          out=SVbig[:, sl_], data0=Abig[:, sl_], data1=Mbig[:, sl_], initial=0.0,
                          op0=ALU.mult, op1=ALU.add),
                          reads=ks("A") + ks("M") + ks("SV"), writes=ks("SV"))
                      S.op("vector", lambda e: e.tensor_copy(out=hcar, in_=SVbig[:, 1023:1024]),
                           reads=ks("SV"), writes=[("hcar",)])
                  else:
                      S.op("vector", lambda e: e.tensor_tensor_scan(
                          out=SVbig[:, sl_], data0=Abig[:, sl_], data1=Mbig[:, sl_], initial=hcar,
                          op0=ALU.mult, op1=ALU.add),
                          reads=ks("A") + ks("M") + ks("SV") + [("hcar",)], writes=ks("SV"))
                  S.op("vector", lambda e: e.scalar_tensor_tensor(
                      out=cc[:, 4 + c, sl_], in0=SVbig[:, sl_], scalar=lnwh[:, c:c + 1], in1=szls[c % 2][:, sl_],
                      op0=ALU.mult, op1=ALU.mult),
                      reads=ks("SV") + [("szl", c % 2, 2 * hf), ("szl", c % 2, 2 * hf + 1), ("halfcols", "lnw")],
                      writes=[("cc", 4 + c, 2 * hf), ("cc", 4 + c, 2 * hf + 1)])
                  if hf == 1 and ("hl%d" % l) in dbg:
                      dump_part("hl%d" % l, c, 0, SVbig[:, 0:1024], allk("SV"))
                      dump_part("hl%d" % l, c, 1, SVbig[:, 1024:2048], allk("SV"))

              def stage_sq(c, hf):
                  sl_ = slice(hf * 1024, (hf + 1) * 1024)
                  S.op("scalar", lambda e: e.activation(out=hsq[:, sl_], in_=SVbig[:, sl_], func=AF.Square),
                       reads=[("SV", 2 * hf), ("SV", 2 * hf + 1)], writes=[("hsq", hf)])

              def stage_ssq(c):
                  for t16 in range(16):
                      S.op("tensor", lambda e, t16=t16: e.matmul(
                          ps[6][:, t16:t16 + 1], lhsT=hsq[:, t16 * 128:(t16 + 1) * 128], rhs=ones_bf[:, 0:1],
                          start=True, stop=True),
                          reads=[("hsq", 0), ("hsq", 1), ("ones_bf",)], writes=[("ps", 6)])
                  if c == 0:
                      S.op("vector", lambda e: e.tensor_copy(out=ssql, in_=ps[6][:, 0:16]),
                           reads=[("ps", 6)], writes=[("ssql", 0), ("ssql", 1)])
                  else:
                      S.op("vector", lambda e: e.tensor_tensor(out=ssql, in0=ssql, in1=ps[6][:, 0:16], op=ALU.add),
                           reads=[("ps", 6), ("ssql", 0), ("ssql", 1)], writes=[("ssql", 0), ("ssql", 1)])

              Vt = U[:, 14336:22656].rearrange("p (t h d) -> p t h d", t=16, h=8)
              lru_alias = ([("xlb", b_) for b_ in (0, 1)] + [("xlb", b_, "h") for b_ in (0, 1)] + [("xcb", b_) for b_ in (0, 1)]
                           + [("rt", b_) for b_ in (0, 1)] + [("itv",)] + [("dg", b_) for b_ in (0, 1)]
                           + [("wxb", b_) for b_ in (0, 1)] + [("cbrow", b_) for b_ in (0, 1)])

              def v_proj():
                  wv = stg[0][:, 0:4096].rearrange("p (k c) -> p k c", k=8)
                  S.op("gpsimd", lambda e: e.memset(Vt[:, :, :, 64:65], 1.0), writes=[("Vones",)] + lru_alias)
                  for tt in range(NT):
                      pv = ps[tt % 6]
                      for k in range(8):
                          S.op("tensor", lambda e, k=k, tt=tt, pv=pv: e.matmul(
                              pv[:, :], lhsT=hT[:, k, tt * 128:(tt + 1) * 128], rhs=wv[:, k, :], start=(k == 0), stop=(k == 7)),
                              reads=[("hT", tt), ("stg", 0)], writes=[("ps", tt % 6)])
                      S.op("scalar" if tt % 2 else "vector", (lambda e, tt=tt, pv=pv: e.activation(
                          out=Vt[:, tt, :, 0:64], in_=pv[:, :].rearrange("p (h d) -> p h d", h=8), func=AF.Copy)) if tt % 2 else
                          (lambda e, tt=tt, pv=pv: e.tensor_copy(
                              out=Vt[:, tt, :, 0:64], in_=pv[:, :].rearrange("p (h d) -> p h d", h=8))),
                          reads=[("ps", tt % 6)], writes=[("V", tt)] + lru_alias)

              units = [(c, t) for c in range(4) for t in range(4)]
              lru_load(0)
              lru_load(1)
              part1_mm(0, 0)
              part1_mm(0, 1)
              premm.update([(0, 0), (0, 1)])
              S.barrier()
              done1 = set()

              def p1(ui):
                  if ui < len(units) and ui not in done1:
                      done1.add(ui)
                      part1(*units[ui])

              p1(0)
              for ui, (c, t) in enumerate(units):
                  p1(ui + 1)
                  if c > 0 and t == 0:
                      stage_sq(c - 1, 0)
                  if c > 0 and t == 2:
                      stage_sq(c - 1, 1)
                      stage_ssq(c - 1)
                  part2(c, t)
                  if c > 0 and t == 0:
                      stage_c(c - 1, 1)
                  if c == 3 and t == 1:
                      S.dma("gpsimd", "stg0", stg[0][:, 0:4096].rearrange("p (k c) -> p k c", k=8), win_v[:, :, 1024:1536],
                            writes=[("stg", 0), ("stgz", 0)] + [("szl", 0, t_) for t_ in range(4)])
                  if t == 3:
                      stage_b(c)
                      p1(ui + 2)
                      stage_c(c, 0)
                      if c == 3:
                          stage_c(c, 1)
                          stage_sq(c, 0)
                          stage_sq(c, 1)
                          v_proj()
                      if c + 2 < 4:
                          lru_load(c + 2)
              stage_ssq(3)
              dump("cclru%d" % l, cc[:, 4:8, :], [128, 4, S_LEN], BF16,
                   [("cc", 4 + c, g) for c in range(4) for g in range(4)])
              dump("ssql%d" % l, ssql, [128, 16], F32, [("ssql", 0), ("ssql", 1)])
              S.barrier()
              if "stopP1" in dbg:
                  break

              qk = {("q", 0): U[:, 0:2048], ("k", 0): U[:, 2048:4096],
                    ("q", 1): U[:, 4096:6144], ("k", 1): U[:, 6144:8192]}
              szT = U[:, 8192:10240]
              gp = U[:, 10240:12288].rearrange("p (t f) -> p t f", t=16)
              PT = [mod_bc.bitcast(BF16)[:, i * 512:(i + 1) * 512] for i in range(4)]
              mt = U[:, 13312:13888].rearrange("p (j c) -> p j c", j=8)
              g8 = Uf[:, 6944:7008]
              top8 = Uf[:, 7008:7072]
              ltm = Uf[:, 7072:7136]
              kmf = Uf[:, 7136:7144]
              kmb = U[:, 14288:14296]
              junk64f = Uf[:, 11328:11392]
              ssqp = smc("ssqp", 128)
              rden = smc("rden8", 8)
              S.op("vector", lambda e: e.memset(qk[("q", 1)][0:64, :], 0.0), writes=[("qk", "q", 1, g) for g in range(4)])
              S.op("vector", lambda e: e.memset(qk[("k", 1)][0:64, :], 0.0), writes=[("qk", "k", 1, g) for g in range(4)])
              S.op("vector", lambda e: e.memset(qk[("q", 0)][64:72, :], 0.0), writes=[("qk", "q", 0, g) for g in range(4)])
              S.op("vector", lambda e: e.memset(U[:, 13312:13888], 0.0), writes=[("mt",)])
              S.dma("sync", "kr0", qk[("k", 0)][64:73, :], krows_d, writes=[("qk", "k", 0, g) for g in range(4)])
              S.dma("sync", "kr1", qk[("k", 1)][0:9, :], krows_d, writes=[("qk", "k", 1, g) for g in range(4)])
              S.dma("sync", "qo0", qk[("q", 0)][73:74, :], krows_d[8:9, :], writes=[("qk", "q", 0, g) for g in range(4)])
              S.dma("sync", "qo1", qk[("q", 1)][9:10, :], krows_d[8:9, :], writes=[("qk", "q", 1, g) for g in range(4)])
              S.dma("sync", "qo2", qk[("q", 0)][75:76, :], krows_d[8:9, :], writes=[("qk", "q", 0, g) for g in range(4)])
              S.dma("sync", "qo3", qk[("q", 1)][11:12, :], krows_d[8:9, :], writes=[("qk", "q", 1, g) for g in range(4)])
              S.dma("sync", "ko2", qk[("k", 0)][74:75, :], krows_d[8:9, :], writes=[("qk", "k", 0, g) for g in range(4)])
              S.dma("sync", "ko3", qk[("k", 1)][10:11, :], krows_d[8:9, :], writes=[("qk", "k", 1, g) for g in range(4)])
              wo_t = [stg[0][:, 0:4096].rearrange("p (c d) -> p c d", c=4), stg[1][:, 0:4096].rearrange("p (c d) -> p c d", c=4)]

              def wo_ap(ch, lo=0, hi=D):
                  return wo_t[ch // 4][:, ch % 4, lo:hi]

              def wo_load(c0, c1, extra_keys):
                  for ch in range(c0, c1):
                      S.dma("gpsimd", "wo%d" % (ch % 2), wo_ap(ch), wout_d[l, ch * 128:(ch + 1) * 128, :],
                            writes=[("wo", ch)] + extra_keys)

              def wo_scale(c0, c1):
                  for ch in range(c0, c1):
                      S.op("vector", lambda e, ch=ch: e.tensor_tensor(
                          out=wo_ap(ch), in0=wo_ap(ch), in1=mod_bc[:, 2048:3072], op=ALU.mult),
                          reads=[("wo", ch), ("mod", 4), ("mod", 5)], writes=[("wo", ch)])

              for p in range(4):
                  wp = stg[1][:, 0:3072].rearrange("p (k c) -> p k c", k=8)
                  S.dma("gpsimd", "stg1", wp[:, :, 0:128], win_v[:, :, p * 128:(p + 1) * 128], writes=[("stgp", 0)])
                  S.dma("gpsimd", "stg1b", wp[:, :, 128:256], win_v[:, :, 512 + p * 128:512 + (p + 1) * 128], writes=[("stgp", 1)])
                  S.dma("gpsimd", "stg1c", wp[:, :, 256:384], win_v[:, :, 1536 + p * 128:1536 + (p + 1) * 128], writes=[("stgp", 2)])
                  S.dma("sync", "ar0", qk[("q", 0)][72:73, :], arow_d[2 * p:2 * p + 1, :], writes=[("qk", "q", 0, g) for g in range(4)])
                  S.dma("sync", "ar1", qk[("q", 1)][8:9, :], arow_d[2 * p + 1:2 * p + 2, :], writes=[("qk", "q", 1, g) for g in range(4)])
                  S.dma("sync", "ka0", qk[("k", 0)][73:74, :], karow_d[2 * p:2 * p + 1, :], writes=[("qk", "k", 0, g) for g in range(4)])
                  S.dma("sync", "ka1", qk[("k", 1)][9:10, :], karow_d[2 * p + 1:2 * p + 2, :], writes=[("qk", "k", 1, g) for g in range(4)])
                  S.dma("sync", "ah0", qk[("q", 0)][74:75, :], arowhi_d[2 * p:2 * p + 1, :], writes=[("qk", "q", 0, g) for g in range(4)])
                  S.dma("sync", "ah1", qk[("q", 1)][10:11, :], arowhi_d[2 * p + 1:2 * p + 2, :], writes=[("qk", "q", 1, g) for g in range(4)])
                  S.dma("sync", "kh0", qk[("k", 0)][75:76, :], karowhi_d[2 * p:2 * p + 1, :], writes=[("qk", "k", 0, g) for g in range(4)])
                  S.dma("sync", "kh1", qk[("k", 1)][11:12, :], karowhi_d[2 * p + 1:2 * p + 2, :], writes=[("qk", "k", 1, g) for g in range(4)])
                  pi_ = [0]

                  PB = [0, 1, 4, 5]

                  def proj(which, wi, tg):
                      pq = ps[PB[pi_[0] % 4]]
                      pkey = ("ps", PB[pi_[0] % 4])
                      pi_[0] += 1
                      hkeys = [("hT", 4 * tg + j) for j in range(4)]
                      for k in range(8):
                          S.op("tensor", lambda e, k=k: e.matmul(
                              pq[:, :], lhsT=wp[:, k, wi * 128:(wi + 1) * 128], rhs=hT[:, k, tg * 512:(tg + 1) * 512],
                              start=(k == 0), stop=(k == 7)),
                              reads=hkeys + [("stgp", wi)], writes=[pkey])
                      if which in "qk":
                          S.op("vector", lambda e: e.tensor_copy(
                              out=qk[(which, 0)][0:64, tg * 512:(tg + 1) * 512], in_=pq[0:64, :]),
                              reads=[pkey], writes=[("qk", which, 0, tg)])
                          S.op("scalar", lambda e: e.activation(
                              out=qk[(which, 1)][64:128, tg * 512:(tg + 1) * 512], in_=pq[64:128, :], func=AF.Copy),
                              reads=[pkey], writes=[("qk", which, 1, tg)])
                      else:
                          S.op("scalar", lambda e: e.activation(
                              out=szT[:, tg * 512:(tg + 1) * 512], in_=pq[:, :], func=AF.Tanh, scale=0.5),
                              reads=[pkey], writes=[("szT", tg)])
                          S.op("vector", lambda e: e.scalar_tensor_tensor(
                              out=szT[:, tg * 512:(tg + 1) * 512], in0=szT[:, tg * 512:(tg + 1) * 512], scalar=1.0,
                              in1=pq[:, :], op0=ALU.add, op1=ALU.mult),
                              reads=[pkey, ("szT", tg)], writes=[("szT", tg)])

                  def kmean(hb):
                      r0 = 64 * hb
                      kh = qk[("k", hb)]
                      kkeys = [("qk", "k", hb, g) for g in range(4)]
                      S.op("vector", lambda e: e.tensor_reduce(
                          out=kmf[r0:r0 + 64, :], in_=kh[r0:r0 + 64, :].rearrange("p (n t) -> p n t", n=8), axis=AX.X, op=ALU.add),
                          reads=kkeys, writes=[("kmf", hb)])
                      S.op("vector", lambda e: e.tensor_scalar(
                          out=kmb[r0:r0 + 64, :], in0=kmf[r0:r0 + 64, :], scalar1=1.0 / 256, scalar2=None, op0=ALU.mult),
                          reads=[("kmf", hb)], writes=[("kmb", hb)])

                  def gates(hb):
                      r0 = 64 * hb
                      qh = qk[("q", hb)]
                      for j in range(8):
                          S.op("tensor", lambda e, j=j: e.matmul(
                              ps[2][:, j * 8:(j + 1) * 8], lhsT=qh[r0:r0 + 64, 1024 + j * 128:1024 + (j + 1) * 128],
                              rhs=kmb[r0:r0 + 64, :], start=True, stop=True),
                              reads=[("qk", "q", hb, 2 + j // 4), ("kmb", hb)], writes=[("ps", 2)])

                  def topk(hb):
                      S.op("vector", lambda e: e.tensor_tensor(out=g8, in0=ps[2][:, 0:64], in1=gmask[:, :], op=ALU.add),
                           reads=[("ps", 2), ("gmask",)], writes=[("g8",)])
                      for j in range(8):
                          S.op("vector", lambda e, j=j: e.max(out=top8[:, j * 8:(j + 1) * 8], in_=g8[:, j * 8:(j + 1) * 8]),
                               reads=[("g8",)], writes=[("top8", j)])
                      thr_b = bass.AP(top8.tensor, top8.offset + 2, [[top8.ap[0][0], 128], [8, 8], [0, 8]])
                      S.op("vector", lambda e: e.tensor_tensor(
                          out=ltm.rearrange("p (j n) -> p j n", j=8), in0=g8.rearrange("p (j n) -> p j n", j=8),
                          in1=thr_b, op=ALU.is_lt),
                          reads=[("g8",)] + [("top8", j) for j in range(8)], writes=[("ltm",)])
                      S.op("vector", lambda e: e.tensor_tensor(
                          out=mt[:, :, 64:72], in0=ltm.rearrange("p (j n) -> p j n", j=8),
                          in1=vneg[:, :].rearrange("p (j n) -> p j n", j=8), op=ALU.mult),
                          reads=[("ltm",), ("vneg",)], writes=[("mt",)])

                  def masks_T(hb):
                      qh = qk[("q", hb)]
                      a0 = 64 if hb == 0 else 0
                      for j in range(8):
                          if hb == 0:
                              S.op("tensor", lambda e, j=j: e.transpose(
                                  out=psb[3][0:72, j * 128:(j + 1) * 128], in_=mt[:, j, 0:72], identity=ident_bf[:, :]),
                                  reads=[("mt",), ("ident_bf",)], writes=[("ps", 3)])
                          else:
                              S.op("tensor", lambda e, j=j: e.transpose(
                                  out=psb[3][0:8, j * 128:(j + 1) * 128], in_=mt[:, j, 64:72], identity=ident_bf[:, :]),
                                  reads=[("mt",), ("ident_bf",)], writes=[("ps", 3)])
                      S.op("vector", lambda e: e.tensor_copy(
                          out=qh[a0:a0 + 8, 1024:2048], in_=psb[3][a0:a0 + 8, 0:1024]),
                          reads=[("ps", 3)], writes=[("qk", "q", hb, 2), ("qk", "q", hb, 3)])

                  if p == 1:
                      wo_load(0, 4, [("stg", 0)])
                  if p == 2:
                      wo_scale(0, 4)
                  for tg in range(4):
                      proj("k", 1, tg)
                  kmean(0)
                  kmean(1)
                  for tg in range(4):
                      proj("q", 0, tg)
                  gates(0)
                  topk(0)
                  proj("z", 2, 0)
                  proj("z", 2, 1)
                  masks_T(0)
                  gates(1)
                  topk(1)
                  proj("z", 2, 2)
                  proj("z", 2, 3)
                  masks_T(1)
                  if p == 3:
                      wo_load(4, 8, [("stgp", 0), ("stgp", 1), ("stgp", 2)])
                  stop_at("stopProj")
                  stop_at("stopMask")
                  for hb in range(2):
                      h = 2 * p + hb
                      r0 = 64 * hb
                      qh = qk[("q", hb)]
                      kh = qk[("k", hb)]
                      K1 = 76 if hb == 0 else 128
                      steps = [(G, kt) for G in range(4) for kt in range(4 * G + 4)]
                      LOOK = 3
                      SB = [4, 5, 0, 1]

                      def geom(i):
                          G, kt = steps[i]
                          rel = kt // 2 - 2 * G
                          if rel < 0:
                              return G, kt, 0, None
                          c0 = 256 * rel + 128 * (kt % 2)
                          return G, kt, c0, c0

                      def emit_S(i, qh=qh, kh=kh, K1=K1, hb=hb):
                          G, kt, c0, tri = geom(i)
                          st_, skey = ps[SB[i % 4]], ("ps", SB[i % 4])
                          S.op("tensor", lambda e: e.matmul(
                              st_[:, c0:512], lhsT=kh[0:K1, kt * 128:(kt + 1) * 128],
                              rhs=qh[0:K1, G * 512 + c0:(G + 1) * 512], start=True, stop=(tri is None)),
                              reads=[("qk", "k", hb, kt // 4), ("qk", "q", hb, G)], writes=[skey])
                          if tri is not None:
                              S.op("tensor", lambda e: e.matmul(
                                  st_[:, tri:tri + 128], lhsT=ident_bf[:, :], rhs=trimask[:, :], start=False, stop=True),
                                  reads=[("ident_bf",), ("trimask",)], writes=[skey])

                      def emit_E(i):
                          G, kt, c0, tri = geom(i)
                          st_, skey = ps[SB[i % 4]], ("ps", SB[i % 4])
                          S.op("scalar", lambda e: e.activation(
                              out=PT[i % 4][:, c0:512], in_=st_[:, c0:512], func=AF.Exp, scale=0.125),
                              reads=[skey], writes=[("PT", i % 4)])

                      def emit_PV(i, h=h, hb=hb):
                          G, kt, c0, tri = geom(i)
                          accb = ps[6 + G % 2]
                          akey = ("acc", G % 2)
                          for qt in range(c0 // 128, 4):
                              lastkt = 4 * G + qt
                              S.op("tensor", lambda e, qt=qt: e.matmul(
                                  accb[:, qt * 128:qt * 128 + 65], lhsT=PT[i % 4][:, qt * 128:(qt + 1) * 128],
                                  rhs=Vt[:, kt, h, :], start=(kt == 0 and qt == 0), stop=(kt == lastkt),
                                  skip_group_check=True),
                                  reads=[("PT", i % 4), ("V", kt), ("Vones",)], writes=[akey])
                          if kt == 4 * G + 3:
                              rd = rden[:, 4 * (G % 2):4 * (G % 2) + 4]
                              S.op("vector", lambda e: e.reciprocal(
                                  out=rd, in_=accb[:, 0:512].rearrange("p (t c) -> p t c", t=4)[:, :, 64]),
                                  reads=[akey], writes=[("rden", G % 2)])
                              for qt in range(4):
                                  tt = 4 * G + qt
                                  S.op("vector", lambda e, qt=qt, tt=tt: e.tensor_scalar(
                                      out=gp[:, tt, 64 * hb:64 * hb + 64], in0=accb[:, qt * 128:qt * 128 + 64],
                                      scalar1=rd[:, qt:qt + 1], scalar2=None, op0=ALU.mult),
                                      reads=[akey, ("rden", G % 2)], writes=[("gp", tt, hb)])
                                  S.op("vector", lambda e, qt=qt, tt=tt: e.scalar_tensor_tensor(
                                      out=junk64f, in0=accb[:, qt * 128:qt * 128 + 64], scalar=rd[:, qt:qt + 1],
                                      in1=gp[:, tt, 64 * hb:64 * hb + 64], op0=ALU.mult, op1=ALU.mult,
                                      accum_out=ssqp[:, h * 16 + tt:h * 16 + tt + 1]),
                                      reads=[akey, ("rden", G % 2), ("gp", tt, hb)], writes=[("junk64",), ("ssqp", h, tt)])

                      for i in range(len(steps) + LOOK):
                          if i < len(steps):
                              emit_S(i)
                          j = i - LOOK
                          if j >= 0:
                              emit_E(j)
                              emit_PV(j)
                  stop_at("stopAttnPair")
                  for tg in range(4):
                      tb = 3 if tg % 2 == 0 else 2
                      for j in range(4):
                          tt = 4 * tg + j
                          S.op("tensor", lambda e, tt=tt, j=j, tb=tb: e.transpose(
                              out=psb[tb][:, j * 128:(j + 1) * 128], in_=gp[:, tt, :], identity=ident_bf[:, :]),
                              reads=[("gp", tt, 0), ("gp", tt, 1), ("ident_bf",)], writes=[("ps", tb)])
                      S.op("vector", lambda e, tg=tg, p=p, tb=tb: e.scalar_tensor_tensor(
                          out=cc[:, p, tg * 512:(tg + 1) * 512], in0=psb[tb][:, 0:512], scalar=anwh[:, p:p + 1],
                          in1=szT[:, tg * 512:(tg + 1) * 512], op0=ALU.mult, op1=ALU.mult),
                          reads=[("ps", tb), ("szT", tg), ("halfcols", "anw")], writes=[("cc", p, tg)])
              ssqa = smc("ssqa", 16)
              S.op("vector", lambda e: e.tensor_reduce(
                  out=ssqa, in_=ssqp.rearrange("p (h t) -> p t h", h=8), axis=AX.X, op=ALU.add),
                  reads=[("ssqp", h_, t_) for h_ in range(8) for t_ in range(16)], writes=[("ssqa",)])
              dump("ccattn%d" % l, cc[:, 0:4, :], [128, 4, S_LEN], BF16,
                   [("cc", c, g) for c in range(4) for g in range(4)])
              dump("ssqa%d" % l, ssqa, [128, 16], F32, [("ssqa",)])
              S.barrier()
              if "stopP2" in dbg:
                  break

              fnw_bc = Uf[:, 4096:5120]
              outt = [Uf[:, 5120:6144], Uf[:, 6144:7168]]
              junk3 = U[:, 14336:15360]
              rstda = smc("rstda", 16)
              rstdl = smc("rstdl", 16)
              for (dst, src_, key) in ((rstda, ssqa, ("ssqa",)), (rstdl, ssql, None)):
                  rk = [key] if key else [("ssql", 0), ("ssql", 1)]
                  S.op("gpsimd", lambda e, dst=dst, src_=src_: e.tensor_scalar(
                      out=dst, in0=src_, scalar1=1.0 / 512, scalar2=EPS, op0=ALU.mult, op1=ALU.add),
                      reads=rk, writes=[("rstd", id(dst))])
                  S.op("gpsimd", lambda e, dst=dst: e.tensor_tensor(
                      out=dst, in0=dst, in1=bass.AP(mhalf.tensor, mhalf.offset, [[mhalf.ap[0][0], 128], [0, 16]]), op=ALU.pow),
                      reads=[("rstd", id(dst)), ("mhalf",)], writes=[("rstd", id(dst))])
              rstd_keys = [("rstd", id(rstda)), ("rstd", id(rstdl))]
              wo_scale(4, 8)
              if final and last_layer:
                  fsrc = bass.AP(fnw_d.tensor, 0, [[0, 128], [1, D]])
                  S.dma("sync", "fnw", fnw_bc, fsrc, writes=[("fnw",)])
                  ssqf = smc("ssqf", 16)
                  rstdf = smc("rstdf", 16)
              def final_out(t_):
                  ot = outt[t_ % 2]
                  S.op("vector", lambda e: e.scalar_tensor_tensor(
                      out=ot, in0=x_sb[:, t_, :], scalar=rstdf[:, t_:t_ + 1], in1=fnw_bc, op0=ALU.mult, op1=ALU.mult),
                      reads=[("x", t_), ("rstdf", t_), ("fnw",)], writes=[("outt", t_ % 2)])
                  tok = S.dma("sync", "xout%d" % (t_ % 2), out_d[t_ * 128:(t_ + 1) * 128, :], ot,
                              reads=[("outt", t_ % 2)])
                  S.wait_at_end("sync", tok)

              cckeys = lambda tt: [("cc", ch, tt // 4) for ch in range(8)]
              it = 0
              nxt = (not last_layer)
              if nxt:
                  mprep_n = mod_prep(16384, 17408)
              for tt in range(NT):
                  if nxt and tt in (0, 2, 6, 8, 10, 12):
                      mod_dma(layers[li + 1], (0, 2, 6, 8, 10, 12).index(tt), True)
                  if nxt and tt % 2 == 1 and tt >= 5 and (tt - 5) // 2 < 6:
                      mod_mm(layers[li + 1], (tt - 5) // 2, mprep_n, True)
                  for dh in range(2):
                      nset = 2 if nxt else 3
                      pa = ps[2 * (it % nset)]
                      pl = ps[2 * (it % nset) + 1]
                      ka = ("ps", 2 * (it % nset))
                      kl = ("ps", 2 * (it % nset) + 1)
                      it += 1
                      for ch in range(4):
                          S.op("tensor", lambda e, ch=ch, tt=tt, dh=dh, pa=pa: e.matmul(
                              pa[:, :], lhsT=cc[:, ch, tt * 128:(tt + 1) * 128], rhs=wo_ap(ch, dh * 512, (dh + 1) * 512),
                              start=(ch == 0), stop=(ch == 3)),
                              reads=[("cc", ch, tt // 4), ("wo", ch)], writes=[ka])
                      for ch in range(4, 8):
                          S.op("tensor", lambda e, ch=ch, tt=tt, dh=dh, pl=pl: e.matmul(
                              pl[:, :], lhsT=cc[:, ch, tt * 128:(tt + 1) * 128], rhs=wo_ap(ch, dh * 512, (dh + 1) * 512),
                              start=(ch == 4), stop=(ch == 7)),
                              reads=[("cc", ch, tt // 4), ("wo", ch)], writes=[kl])
                      xs = x_sb[:, tt, dh * 512:(dh + 1) * 512]
                      S.op("vector", lambda e, xs=xs, pa=pa, tt=tt: e.scalar_tensor_tensor(
                          out=xs, in0=pa[:, :], scalar=rstda[:, tt:tt + 1], in1=xs, op0=ALU.mult, op1=ALU.add),
                          reads=[ka, ("x", tt)] + rstd_keys, writes=[("x", tt)])
                      S.op("vector", lambda e, xs=xs, pl=pl, tt=tt: e.scalar_tensor_tensor(
                          out=xs, in0=pl[:, :], scalar=rstdl[:, tt:tt + 1], in1=xs, op0=ALU.mult, op1=ALU.add),
                          reads=[kl, ("x", tt)] + rstd_keys, writes=[("x", tt)])
                  if nxt:
                      x_stats(tt, junk3)
                  if final and last_layer:
                      S.op("scalar", lambda e, tt=tt: e.activation(
                          out=junk3, in_=x_sb[:, tt, :], func=AF.Square, accum_out=ssqf[:, tt:tt + 1]),
                          reads=[("x", tt)], writes=[("junk3",), ("ssqf", tt)])
                      S.op("gpsimd", lambda e, tt=tt: e.tensor_scalar(
                          out=rstdf[:, tt:tt + 1], in0=ssqf[:, tt:tt + 1], scalar1=1.0 / D, scalar2=EPS,
                          op0=ALU.mult, op1=ALU.add),
                          reads=[("ssqf", tt)], writes=[("rstdf", tt)])
                      S.op("gpsimd", lambda e, tt=tt: e.tensor_tensor(
                          out=rstdf[:, tt:tt + 1], in0=rstdf[:, tt:tt + 1], in1=mhalf, op=ALU.pow),
                          reads=[("rstdf", tt), ("mhalf",)], writes=[("rstdf", tt)])
                      if tt >= 2:
                          final_out(tt - 2)
              if final and last_layer:
                  final_out(NT - 2)
                  final_out(NT - 1)
              elif last_layer:
                  for tt in range(NT):
                      tok = S.dma("sync", "xout%d" % (tt % 4), out_d[tt * 128:(tt + 1) * 128, :], x_sb[:, tt, :],
                                  reads=[("x", tt)])
                      S.wait_at_end("sync", tok)
              dump("xend%d" % l, x_sb[:, :, :], [128, NT, D], F32, [("x", t) for t in range(NT)])
              S.barrier()


        except _Stop:
            pass

        for sname, n in S.dma_cnt.items():
            S.wait_at_end("sync", ("d_" + sname, 16 * n))
        run = S.emit_all(None)
        with nc.Block() as block:
            @block.sync
            def _(e):
                run("sync", e)

            @block.scalar
            def _(e):
                run("scalar", e)

            @block.vector
            def _(e):
                run("vector", e)

            @block.gpsimd
            def _(e):
                run("gpsimd", e)

            @block.tensor
            def _(e):
                run("tensor", e)
    return nc, list(dbg_d.keys())


def make_inmaps(inp, L_total, x_override=None):
    consts = host_consts()
    inp = {k: np.asarray(v) for k, v in inp.items()}
    rab = blockdiag(inp["rg_a_w"].astype(np.float32))
    rxb = blockdiag(inp["rg_x_w"].astype(np.float32))
    xs = inp["x"] if x_override is None else x_override
    maps = []
    for b in range(8):
        m = {
            "x": np.ascontiguousarray(xs[b], dtype=np.float32),
            "cols": pack_cols(inp, b, L_total),
            "ada_w": np.ascontiguousarray(inp["ada_w"], dtype=np.float32),
            "ada_b": np.ascontiguousarray(inp["ada_b"], dtype=np.float32),
            "w_in": np.ascontiguousarray(inp["w_in"], dtype=np.float32),
            "w_out": np.ascontiguousarray(inp["w_out"], dtype=np.float32),
            "rg_a_bd": rab,
            "rg_x_bd": rxb,
            "final_norm_w": np.ascontiguousarray(inp["final_norm_w"], dtype=np.float32),
            "conv_b": np.ascontiguousarray(inp["conv_b"], dtype=np.float32),
        }
        m.update(consts)
        maps.append(m)
    return maps


_CACHE = {}


def kernel(**inputs):
    L_total = 2
    maps = make_inmaps(inputs, L_total)
    key = ("full",)
    if key not in _CACHE:
        _CACHE[key] = build([0, 1], True, L_total)[0]
    nc = _CACHE[key]
    res = run_bass_kernel_spmd(nc, maps, core_ids=list(range(8)))
    out = np.stack([np.asarray(r["out"], dtype=np.float32) for r in res.results], axis=0)
    return out
```

```python
import contextlib
import numpy as np
import ml_dtypes
import concourse.bass as bass
import concourse.mybir as mybir
from concourse.bass_utils import run_bass_kernel_spmd

F32 = mybir.dt.float32
BF16 = mybir.dt.bfloat16
AF = mybir.ActivationFunctionType
ALU = mybir.AluOpType
AX = mybir.AxisListType

S_LEN = 2048
D = 1024
NT = 16
H = 8
EPS = 1e-6
NEG = -30000.0
ENGS = ["sync", "scalar", "vector", "gpsimd", "tensor"]


class _Stop(Exception):
    pass


class Sched:
    def __init__(self, nc, stack):
        self.nc = nc
        self.stack = stack
        self.prog = {e: [] for e in ENGS}
        self.cnt = {e: 0 for e in ENGS}
        self.sems = {}
        self.seen = {e: {} for e in ENGS}
        self.last_w = {}
        self.readers = {}
        self.dma_cnt = {}
        self.pending = {e: {} for e in ENGS}
        self.waited = set()
        self.final = {e: {} for e in ENGS}

    def sem(self, name):
        if name not in self.sems:
            self.sems[name] = self.stack.enter_context(self.nc.semaphore(name))
        return self.sems[name]

    def _collect(self, eng, reads, writes, extra=()):
        waits = dict(self.pending[eng])
        self.pending[eng] = {}

        def need(tok):
            if tok is None:
                return
            s, v = tok
            if v > waits.get(s, 0):
                waits[s] = v

        for k in reads:
            need(self.last_w.get(k))
        for k in writes:
            need(self.last_w.get(k))
            for s, v in self.readers.get(k, {}).items():
                need((s, v))
        for t in extra:
            need(t)
        wl = []
        for s, v in waits.items():
            if eng == "tensor" and s == "e_tensor":
                continue
            if self.seen[eng].get(s, 0) >= v:
                continue
            self.seen[eng][s] = v
            self.waited.add((s, v))
            wl.append((s, v))
        return wl

    def _commit(self, tok, reads, writes):
        for k in writes:
            self.last_w[k] = tok
            self.readers[k] = {}
        for k in reads:
            r = self.readers.setdefault(k, {})
            if tok[1] > r.get(tok[0], 0):
                r[tok[0]] = tok[1]

    def op(self, eng, fn, reads=(), writes=()):
        wl = self._collect(eng, reads, writes)
        self.cnt[eng] += 1
        tok = ("e_" + eng, self.cnt[eng])
        self.sem(tok[0])
        self.prog[eng].append((wl, fn, tok, False))
        self._commit(tok, reads, writes)
        return tok

    def dma(self, eng, stream, out, in_, reads=(), writes=(), **kw):
        n = self.dma_cnt.get(stream, 0)
        extra = [("d_" + stream, 16 * n)] if n > 0 else []
        wl = self._collect(eng, reads, writes, extra)
        self.dma_cnt[stream] = n + 1
        tok = ("d_" + stream, 16 * (n + 1))
        self.sem(tok[0])
        self.prog[eng].append((wl, (lambda e: e.dma_start(out=out, in_=in_, **kw)), tok, True))
        self._commit(tok, reads, writes)
        return tok

    def barrier(self):
        toks = {}
        for e in ENGS:
            if self.cnt[e] > 0:
                toks["e_" + e] = self.cnt[e]
        for s, n in self.dma_cnt.items():
            toks["d_" + s] = 16 * n
        for e in ENGS:
            for s, v in toks.items():
                if v > self.pending[e].get(s, 0):
                    self.pending[e][s] = v
        self.last_w = {}
        self.readers = {}

    def wait_at_end(self, eng, tok):
        self.final[eng][tok[0]] = max(self.final[eng].get(tok[0], 0), tok[1])
        self.waited.add(tok)

    def emit(self, eng, e):
        remap = {}
        run = 0
        for (wl, fn, tok, is_dma) in self.prog[eng]:
            if not is_dma and tok in self.waited:
                run += 1
                remap[tok[1]] = run
        self._remaps[eng] = remap

    def emit_all(self, block_engines):
        self._remaps = {}
        for eng in ENGS:
            self.emit(eng, None)

        def mapped(s, v):
            if s.startswith("e_"):
                return self._remaps[s[2:]][v]
            return v

        def run(eng, e):
            for (wl, fn, tok, is_dma) in self.prog[eng]:
                for (s, v) in wl:
                    e.wait_ge(self.sems[s], mapped(s, v))
                inst = fn(e)
                if is_dma:
                    inst.then_inc(self.sems[tok[0]], 16)
                elif tok in self.waited:
                    inst.then_inc(self.sems[tok[0]], 1)
            for s, v in self.pending[eng].items():
                pass
            for s, v in self.final[eng].items():
                e.wait_ge(self.sems[s], mapped(s, v))

        return run


def alibi_slopes():
    return [2.0 ** (-8.0 * (i + 1) / H) for i in range(H)]


def host_consts():
    sl = alibi_slopes()
    c = {}
    c["ident_bf"] = np.eye(128, dtype=np.float32).astype(ml_dtypes.bfloat16)
    c["ident_f"] = np.eye(128, dtype=np.float32)
    k = np.arange(128)[:, None]
    q = np.arange(128)[None, :]
    c["trimask"] = np.where(q >= k, 0.0, NEG).astype(np.float32).astype(ml_dtypes.bfloat16)
    bt = np.zeros((128, H, 17), np.float32)
    for h in range(H):
        for j in range(17):
            bt[:, h, j] = sl[h] * (np.arange(128) - 128.0 * (j - 1))
    c["bias_tab"] = bt.reshape(128, H * 17)
    qi = (np.arange(S_LEN) % 256).astype(np.float32)
    c["arow"] = np.stack([-8.0 * sl[h] * qi for h in range(H)]).astype(np.float32).astype(ml_dtypes.bfloat16)
    c["karow"] = np.stack([8.0 * sl[h] * qi for h in range(H)]).astype(np.float32).astype(ml_dtypes.bfloat16)
    qbi = (np.arange(S_LEN) // 256).astype(np.float32)
    c["arow_hi"] = np.stack([-8.0 * sl[h] * 256.0 * qbi for h in range(H)]).astype(np.float32).astype(ml_dtypes.bfloat16)
    c["karow_hi"] = np.stack([8.0 * sl[h] * 256.0 * qbi for h in range(H)]).astype(np.float32).astype(ml_dtypes.bfloat16)
    kr = np.zeros((9, S_LEN), np.float32)
    for n in range(8):
        kr[n, n * 256:(n + 1) * 256] = 1.0
    kr[8, :] = 1.0
    c["krows"] = kr.astype(ml_dtypes.bfloat16)
    gm = np.zeros((128, 8, 8), np.float32)
    for j in range(8):
        qb = 4 + j // 2
        gm[:, j, qb:] = -1e30
    c["gmask"] = gm.reshape(128, 64)
    vn = np.zeros((128, 8, 8), np.float32)
    for j in range(8):
        qb = 4 + j // 2
        vn[:, j, :qb] = NEG
    c["vneg"] = vn.reshape(128, 64)
    c["ones_bf"] = np.ones((128, 128), np.float32).astype(ml_dtypes.bfloat16)
    return c


def col_layout(L):
    off = {}
    n = 0

    def add(name, w):
        nonlocal n
        off[name] = (n, w)
        n += w

    add("cT", 8)
    for l in range(L):
        add(("normw", l), 8)
        add(("convw", l), 16)
        add(("convb", l), 4)
        add(("rab", l), 4)
        add(("rxb", l), 4)
        add(("lam", l), 4)
        add(("anw", l), 4)
        add(("lnw", l), 4)
    return off, n


def pack_cols(inp, b, L):
    off, n = col_layout(L)
    cols = np.zeros((128, n), np.float32)

    def put(name, arr):
        o, w = off[name]
        assert arr.shape == (128, w), (name, arr.shape)
        cols[:, o:o + w] = arr

    put("cT", inp["c"][b].reshape(8, 128).T)
    for l in range(L):
        put(("normw", l), inp["norm_w"][l].reshape(8, 128).T)
        cw = inp["conv_w"][l]
        put(("convw", l), cw.reshape(4, 4, 128).transpose(2, 1, 0).reshape(128, 16))
        for nm, key in (("convb", "conv_b"), ("rab", "rg_a_b"), ("rxb", "rg_x_b"), ("lam", "rg_lambda"),
                        ("anw", "attn_out_norm_w"), ("lnw", "lru_out_norm_w")):
            put((nm, l), inp[key][l].reshape(4, 128).T)
    return cols


def blockdiag(w):
    L = w.shape[0]
    out = np.zeros((L, 4, 128, 128), np.float32)
    for c in range(4):
        out[:, c, 0:64, 0:64] = w[:, 2 * c]
        out[:, c, 64:128, 64:128] = w[:, 2 * c + 1]
    return out


def build(layers, final, L_total, dbg=()):
    nc = bass.Bass("TRN2", target_bir_lowering=False)
    coff, ncol = col_layout(L_total)
    dr = {}

    def din(name, shape, dt=F32):
        dr[name] = nc.dram_tensor(name, list(shape), dt, kind="ExternalInput").ap()
        return dr[name]

    x_d = din("x", [S_LEN, D])
    cols_d = din("cols", [128, ncol])
    adaw_d = din("ada_w", [L_total, D, 3 * D])
    adab_d = din("ada_b", [L_total, 3 * D])
    win_d = din("w_in", [L_total, D, 3 * D])
    wout_d = din("w_out", [L_total, D, D])
    raw_d = din("rg_a_bd", [L_total, 4, 128, 128])
    rxw_d = din("rg_x_bd", [L_total, 4, 128, 128])
    fnw_d = din("final_norm_w", [D])
    din("conv_b", [L_total, 512])
    identbf_d = din("ident_bf", [128, 128], BF16)
    identf_d = din("ident_f", [128, 128])
    trimask_d = din("trimask", [128, 128], BF16)
    biastab_d = din("bias_tab", [128, H * 17])
    arow_d = din("arow", [H, S_LEN], BF16)
    krows_d = din("krows", [9, S_LEN], BF16)
    karow_d = din("karow", [H, S_LEN], BF16)
    arowhi_d = din("arow_hi", [H, S_LEN], BF16)
    karowhi_d = din("karow_hi", [H, S_LEN], BF16)
    gmask_d = din("gmask", [128, 64])
    vneg_d = din("vneg", [128, 64])
    onesbf_d = din("ones_bf", [128, 128], BF16)
    out_d = nc.dram_tensor("out", [S_LEN, D], F32, kind="ExternalOutput").ap()
    dbg_d = {}

    UW = 22784
    with contextlib.ExitStack() as st:
        def sb(name, shape, dt):
            return st.enter_context(nc.sbuf_tensor(name, list(shape), dt))

        x_sb = sb("x_sb", [128, NT, D], F32)
        hT = sb("hT", [128, 8, S_LEN], BF16)
        cc = sb("cc", [128, 8, S_LEN], BF16)
        stg = [sb("stg0", [128, 4096], BF16), sb("stg1", [128, 4096], BF16)]
        mod_bc = sb("mod_bc", [128, 3 * D], F32)
        U = sb("U", [128, UW], BF16)
        Uf = U.bitcast(F32)
        ident_bf = sb("ident_bf_s", [128, 128], BF16)
        ident_f = sb("ident_f_s", [128, 128], F32)
        trimask = sb("trimask_s", [128, 128], BF16)
        bias_tab = sb("bias_tab_s", [128, H * 17], F32)
        gmask = sb("gmask_s", [128, 64], F32)
        vneg = sb("vneg_s", [128, 64], F32)
        ones_bf = sb("ones_bf_s", [128, 128], BF16)
        ones4 = sb("ones4_s", [1, 512], BF16)
        cols = sb("cols_s", [128, ncol], F32)
        sm = sb("small", [128, 512], F32)
        ps = [st.enter_context(nc.psum_tensor("ps%d" % i, [128, 512], F32)) for i in range(8)]
        psb = [p.bitcast(BF16) for p in ps]

        S = Sched(nc, st)

        def col(name, j0=0, w=None):
            o, ww = coff[name]
            if w is None:
                w = ww - j0
            return cols[:, o + j0:o + j0 + w]

        smo = {}
        smn = [0]

        def smc(name, w):
            if name not in smo:
                smo[name] = (smn[0], w)
                smn[0] += w
                assert smn[0] <= 512
            o, ww = smo[name]
            return sm[:, o:o + ww]

        onesrow = ones4[0:1, :]
        S.dma("sync", "c8", onesrow, krows_d[8:9, 0:512], writes=[("onesrow",)])
        qcol = smc("qcol", 1)
        mhalf = smc("mhalf", 1)
        S.op("vector", lambda e: e.memset(mhalf, -0.5), writes=[("mhalf",)])
        S.op("vector", lambda e: e.memset(qcol, 0.25), writes=[("qcol",)])
        S.dma("sync", "c0", cols[:, :], cols_d, writes=[("cols",)])
        S.dma("sync", "c1", ident_bf[:, :], identbf_d, writes=[("ident_bf",)])
        S.dma("sync", "c2", ident_f[:, :], identf_d, writes=[("ident_f",)])
        S.dma("sync", "c3", trimask[:, :], trimask_d, writes=[("trimask",)])
        S.dma("sync", "c4", bias_tab[:, :], biastab_d, writes=[("bias_tab",)])
        S.dma("sync", "c5", gmask[:, :], gmask_d, writes=[("gmask",)])
        S.dma("sync", "c7", vneg[:, :], vneg_d, writes=[("vneg",)])
        S.dma("sync", "c6", ones_bf[:, :], onesbf_d, writes=[("ones_bf",)])

        dbg_cnt = [0]

        def dump(name, ap, shape, dt, reads):
            if name not in dbg:
                return
            t = nc.dram_tensor("dbg_" + name, list(shape), dt, kind="ExternalOutput").ap()
            dbg_d[name] = t
            tok = S.dma("sync", "dbg%d" % dbg_cnt[0], t, ap, reads=reads)
            dbg_cnt[0] += 1
            S.wait_at_end("sync", tok)


        dparts = {}

        def dump_part(name, c, hf, ap, reads):
            if name not in dparts:
                dparts[name] = nc.dram_tensor("dbg_" + name, [128, 4, S_LEN], F32, kind="ExternalOutput").ap()
                dbg_d[name] = dparts[name]
            tok = S.dma("sync", "dbg%d" % dbg_cnt[0], dparts[name][:, c, hf * 1024:(hf + 1) * 1024], ap, reads=reads)
            dbg_cnt[0] += 1
            S.wait_at_end("sync", tok)

        ssqx = smc("ssqx", 16)
        rstdx = smc("rstdx", 16)

        def x_stats(tt, junk_ap):
            S.op("scalar", lambda e: e.activation(
                out=junk_ap, in_=x_sb[:, tt, :], func=AF.Square, accum_out=ssqx[:, tt:tt + 1]),
                reads=[("x", tt)], writes=[("junkx",), ("ssqx", tt)])
            S.op("gpsimd", lambda e: e.tensor_scalar(
                out=rstdx[:, tt:tt + 1], in0=ssqx[:, tt:tt + 1], scalar1=1.0 / D, scalar2=EPS, op0=ALU.mult, op1=ALU.add),
                reads=[("ssqx", tt)], writes=[("rstdx", tt)])
            S.op("gpsimd", lambda e: e.tensor_tensor(
                out=rstdx[:, tt:tt + 1], in0=rstdx[:, tt:tt + 1], in1=mhalf, op=ALU.pow),
                reads=[("rstdx", tt), ("mhalf",)], writes=[("rstdx", tt)])

        def mod_prep(scoff, aboff):
            th = smc("th", 8)
            sc = smc("sc", 8)
            cT = col("cT")
            S.op("scalar", lambda e: e.activation(out=th, in_=cT, func=AF.Tanh, scale=0.5),
                 reads=[("cols",)], writes=[("th",)])
            S.op("vector", lambda e: e.scalar_tensor_tensor(
                out=sc, in0=th, scalar=1.0, in1=cT, op0=ALU.add, op1=ALU.mult),
                reads=[("th",), ("cols",)], writes=[("sc",)])
            S.op("vector", lambda e: e.tensor_scalar(out=sc, in0=sc, scalar1=0.5, scalar2=None, op0=ALU.mult),
                 reads=[("sc",)], writes=[("sc",)])
            screp = U[:, scoff:scoff + 1024].rearrange("p (k m) -> p k m", k=8)
            for k in range(8):
                S.op("vector", lambda e, k=k: e.tensor_scalar(
                    out=screp[:, k, :], in0=ones_bf[:, :], scalar1=sc[:, k:k + 1], scalar2=None, op0=ALU.mult),
                    reads=[("sc",), ("ones_bf",)], writes=[("screp", k)])
            return screp, aboff

        def mod_bufs(in_u):
            if in_u:
                return [U[:, 0:4096], U[:, 4096:8192]], "ustg"
            return [stg[0][:, 0:4096], stg[1][:, 0:4096]], "stg"

        def mod_dma(l_, ng, in_u=False):
            adaw_v = adaw_d[l_].rearrange("(k p) c -> p k c", p=128)
            bufs, kn = mod_bufs(in_u)
            sgv = bufs[ng % 2].rearrange("p (k c) -> p k c", k=8)
            S.dma("gpsimd", "%s%d" % (kn, ng % 2), sgv, adaw_v[:, :, ng * 512:(ng + 1) * 512],
                  writes=[(kn, ng % 2), ("adaload", ng)])

        def mod_mm(l_, ng, mprep, in_u=False):
            screp, aboff = mprep
            bufs, kn = mod_bufs(in_u)
            if ng == 0:
                S.dma("gpsimd", "adab", U[0:1, aboff:aboff + 3 * D], adab_d[l_:l_ + 1, :], writes=[("adab",)],
                      max_dma_last_dim=2048)
            sgv = bufs[ng % 2].rearrange("p (k c) -> p k c", k=8)
            pt = ps[4 + ng % 2]
            pk = ("ps", 4 + ng % 2)
            for k in range(8):
                S.op("tensor", lambda e, k=k: e.matmul(
                    pt[:, :], lhsT=screp[:, k, :], rhs=sgv[:, k, :], start=(k == 0), stop=False),
                    reads=[("screp", k), (kn, ng % 2)], writes=[pk])
            S.op("tensor", lambda e: e.matmul(
                pt[:, :], lhsT=ones_bf[0:1, :], rhs=U[0:1, aboff + ng * 512:aboff + (ng + 1) * 512], start=False, stop=True),
                reads=[("ones_bf",), ("adab",)], writes=[pk])
            S.op("scalar", lambda e: e.activation(
                out=mod_bc[:, ng * 512:(ng + 1) * 512], in_=pt[:, :], func=AF.Copy),
                reads=[pk], writes=[("mod", ng)])

        first = True

        def stop_at(name):
            if name in dbg:
                raise _Stop()

        try:
          for li, l in enumerate(layers):
              last_layer = (li == len(layers) - 1)
              if li == 0:
                  mprep = mod_prep(0, 9216)
                  for ng in range(4):
                      mod_dma(l, ng)
                      mod_mm(l, ng, mprep)
                  mod_dma(l, 4)
                  mod_dma(l, 5)
              tmpd = Uf[:, 1024:1024 + 2048].rearrange("p (j m) -> p j m", j=16)
              identb = bass.AP(ident_f, 0, [[128, 128], [0, 16], [1, 128]])
              S.op("vector", lambda e: e.tensor_tensor(
                  out=tmpd, in0=mod_bc[:, 0:2048].rearrange("p (j m) -> p j m", j=16), in1=identb, op=ALU.mult),
                  reads=[("mod", g) for g in range(4)] + [("ident_f",)], writes=[("tmpd",)])
              col16 = smc("col16", 16)
              S.op("vector", lambda e: e.tensor_reduce(out=col16, in_=tmpd, axis=AX.X, op=ALU.add),
                   reads=[("tmpd",)], writes=[("col16",)])
              s1 = smc("s1", 8)
              S.op("vector", lambda e, l=l: e.scalar_tensor_tensor(
                  out=s1, in0=col16[:, 8:16], scalar=1.0, in1=col(("normw", l)), op0=ALU.add, op1=ALU.mult),
                  reads=[("col16",), ("cols",)], writes=[("s1",)])
              dump("mod%d" % l, mod_bc[:, :], [128, 3 * D], F32, [("mod", g) for g in range(6)])
              dump("s1_%d" % l, s1, [128, 8], F32, [("s1",)])

              junk = U[:, 6144:6144 + 1024]
              if li == 0:
                  for tt in range(NT):
                      if first:
                          S.dma("sync", "xin%d" % (tt % 4), x_sb[:, tt, :], x_d[tt * 128:(tt + 1) * 128, :],
                                reads=[("adaload", 2)], writes=[("x", tt)])
                      x_stats(tt, junk)
              xn = [U[:, 7168:7168 + 1024], U[:, 8192:8192 + 1024]]
              tmpf = [Uf[:, 6144:7168], Uf[:, 7168:8192]]
              s1_b = bass.AP(s1.tensor, s1.offset, [[s1.ap[0][0], 128], [1, 8], [0, 128]])
              sh_b = bass.AP(col16.tensor, col16.offset, [[col16.ap[0][0], 128], [1, 8], [0, 128]])
              shbf = sm.bitcast(BF16)[:, 960:968]
              S.op("vector", lambda e: e.tensor_copy(out=shbf, in_=col16[:, 0:8]), reads=[("col16",)], writes=[("shbf",)])
              shb_b = bass.AP(shbf.tensor, shbf.offset, [[shbf.ap[0][0], 128], [1, 8], [0, 128]])
              for tt in range(NT):
                  xb = xn[tt % 2]
                  S.op("scalar", lambda e, tt=tt, xb=xb: e.activation(
                      out=xb, in_=x_sb[:, tt, :], func=AF.Copy, scale=rstdx[:, tt:tt + 1]),
                      reads=[("x", tt), ("rstdx", tt)], writes=[("xn", tt % 2)])
                  pb = psb[2 + tt % 2]
                  for k in range(8):
                      S.op("tensor", lambda e, k=k, xb=xb, pb=pb: e.transpose(
                          out=pb[:, k * 128:(k + 1) * 128], in_=xb[:, k * 128:(k + 1) * 128], identity=ident_bf[:, :]),
                          reads=[("xn", tt % 2), ("ident_bf",)], writes=[("ps", 2 + tt % 2)])
                  S.op("vector", lambda e, pb=pb, tt=tt: e.tensor_tensor(
                      out=hT[:, :, tt * 128:(tt + 1) * 128], in0=pb[:, 0:1024].rearrange("p (k m) -> p k m", k=8),
                      in1=s1_b, op=ALU.mult),
                      reads=[("ps", 2 + tt % 2), ("s1",)], writes=[("hT", tt)])
                  S.op("vector", lambda e, tt=tt: e.tensor_tensor(
                      out=hT[:, :, tt * 128:(tt + 1) * 128], in0=hT[:, :, tt * 128:(tt + 1) * 128], in1=shb_b, op=ALU.add),
                      reads=[("hT", tt), ("shbf",)], writes=[("hT", tt)])
                  if li == 0 and tt in (9, 14):
                      mod_mm(l, 4 if tt == 9 else 5, mprep)
              dump("hT%d" % l, hT[:, :, :], [128, 8, S_LEN], BF16, [("hT", t) for t in range(NT)])
              dump("xin%d" % l, x_sb[:, :, :], [128, NT, D], F32, [("x", t) for t in range(NT)])
              dump("c16_%d" % l, col16, [128, 16], F32, [("col16",)])
              first = False
              if "stopP0" in dbg:
                  S.barrier()
                  break
              TU = 512
              Abig = Uf[:, 0:2048]
              SVbig = Uf[:, 2048:4096]
              Mbig = Uf[:, 4096:6144]
              szls = [stg[0][:, 2048:4096], stg[1][:, 2048:4096]]
              hsq = mod_bc.bitcast(BF16)[:, 0:2048]
              xlb = [U[:, 14336:14336 + 516], U[:, 14852:14852 + 516]]
              xcbs = [U[:, 15368:15880], U[:, 15880:16392]]
              rts = [Uf[:, 8196:8708], Uf[:, 8708:9220]]
              itv = Uf[:, 9220:9732]
              dgs = [U[:, 19464:19976], U[:, 19976:20488]]
              smb = sm.bitcast(BF16)
              wabs = [smb[:, 700:828], smb[:, 828:956]]
              wxbs = [U[:, 20488:20616], U[:, 20616:20744]]
              cbrow = [U[0:1, 20744:20872], U[0:1, 20872:21000]]
              lam = col(("lam", l))
              e1 = smc("e1", 4)
              nsp = smc("nsp", 4)
              hnsp = smc("hnsp", 4)
              habh = smc("habh", 4)
              hxbh = smc("hxbh", 4)
              lnwh = smc("lnwh", 4)
              anwh = smc("anwh", 4)
              ssql = smc("ssql", 16)
              hcar = smc("hcar", 1)
              S.op("scalar", lambda e, lam=lam: e.activation(out=e1, in_=lam, func=AF.Exp, scale=-1.0),
                   reads=[("cols",)], writes=[("e1",)])
              S.op("scalar", lambda e: e.activation(out=e1, in_=e1, func=AF.Ln, bias=1.0),
                   reads=[("e1",)], writes=[("e1",)])
              S.op("vector", lambda e: e.tensor_scalar(out=nsp, in0=e1, scalar1=-8.0, scalar2=None, op0=ALU.mult),
                   reads=[("e1",)], writes=[("nsp",)])
              S.op("vector", lambda e: e.tensor_scalar(out=hnsp, in0=e1, scalar1=-4.0, scalar2=None, op0=ALU.mult),
                   reads=[("e1",)], writes=[("hnsp",)])
              for (dst, srcn) in ((habh, "rab"), (hxbh, "rxb"), (lnwh, "lnw"), (anwh, "anw")):
                  S.op("vector", lambda e, dst=dst, srcn=srcn, l=l: e.tensor_scalar(
                      out=dst, in0=col((srcn, l)), scalar1=0.5, scalar2=None, op0=ALU.mult),
                      reads=[("cols",)], writes=[("halfcols", srcn)])
              dump("nsp%d" % l, nsp, [128, 4], F32, [("nsp",)])
              win_v = win_d[l].rearrange("(k p) c -> p k c", p=128)
              convb_d2 = dr["conv_b"]

              def lru_load(c):
                  sg = stg[c % 2]
                  wl_ = sg[:, 0:2048].rearrange("p (k c) -> p k c", k=8)
                  S.dma("gpsimd", "stg%d" % (c % 2), wl_[:, :, 0:128], win_v[:, :, 2048 + c * 128:2048 + (c + 1) * 128],
                        writes=[("stg", c % 2)])
                  S.dma("gpsimd", "stgb%d" % (c % 2), wl_[:, :, 128:256], win_v[:, :, 2560 + c * 128:2560 + (c + 1) * 128],
                        writes=[("stgz", c % 2), ("stg", c % 2)])
                  S.dma("gpsimd", "wab%d" % (c % 2), wabs[c % 2], raw_d[l, c], writes=[("wab", c % 2)])
                  S.dma("gpsimd", "wxb%d" % (c % 2), wxbs[c % 2], rxw_d[l, c], writes=[("wxb", c % 2)])
                  S.dma("gpsimd", "cbr%d" % (c % 2), cbrow[c % 2], convb_d2[l:l + 1, c * 128:(c + 1) * 128],
                        writes=[("cbrow", c % 2)])
                  cw = col(("convw", l), c * 4, 4)
                  for j in range(4):
                      S.op("vector", lambda e, j=j, cw=cw, c=c: e.tensor_scalar(
                          out=dgs[c % 2][:, j * 128:(j + 1) * 128], in0=ident_bf[:, :], scalar1=cw[:, j:j + 1], scalar2=None,
                          op0=ALU.mult),
                          reads=[("ident_bf",), ("cols",)], writes=[("dg", c % 2)])

              premm = set()

              def part1_mm(c, t):
                  b = (4 * c + t) % 2
                  wl_ = stg[c % 2][:, 0:2048].rearrange("p (k c) -> p k c", k=8)
                  pxl, pzl = ps[b], ps[2 + b]
                  kxl, kzl = ("ps", b), ("ps", 2 + b)
                  hkeys = [("hT", 4 * t + j) for j in range(4)]
                  for k in range(8):
                      S.op("tensor", lambda e, k=k: e.matmul(
                          pxl[:, :], lhsT=wl_[:, k, 0:128], rhs=hT[:, k, t * TU:(t + 1) * TU], start=(k == 0), stop=(k == 7)),
                          reads=hkeys + [("stg", c % 2)], writes=[kxl])
                  for k in range(8):
                      S.op("tensor", lambda e, k=k: e.matmul(
                          pzl[:, :], lhsT=wl_[:, k, 128:256], rhs=hT[:, k, t * TU:(t + 1) * TU], start=(k == 0), stop=(k == 7)),
                          reads=hkeys + [("stgz", c % 2)], writes=[kzl])

              def part1(c, t):
                  u = 4 * c + t
                  b = u % 2
                  wl_ = stg[c % 2][:, 0:2048].rearrange("p (k c) -> p k c", k=8)
                  xl_ = xlb[b]
                  pxl, pzl, pcv = ps[b], ps[2 + b], ps[4 + b]
                  kxl, kzl, kcv = ("ps", b), ("ps", 2 + b), ("ps", 4 + b)
                  hkeys = [("hT", 4 * t + j) for j in range(4)]
                  if (c, t) not in premm:
                      part1_mm(c, t)
                  if t == 0:
                      S.op("vector", lambda e: e.memset(xl_[:, 0:3], 0.0), writes=[("xlb", b, "h")])
                  else:
                      S.op("vector", lambda e: e.tensor_copy(out=xl_[:, 0:3], in_=xlb[1 - b][:, TU:TU + 3]),
                           reads=[("xlb", 1 - b)], writes=[("xlb", b, "h")])
                  S.op("vector", lambda e: e.tensor_copy(out=xl_[:, 3:3 + TU], in_=pxl[:, :]),
                       reads=[kxl], writes=[("xlb", b)])
                  S.op("scalar", lambda e: e.activation(out=rts[b], in_=pzl[:, :], func=AF.Tanh, scale=0.5),
                       reads=[kzl], writes=[("rt", b)])
                  S.op("vector", lambda e: e.scalar_tensor_tensor(
                      out=szls[c % 2][:, t * TU:(t + 1) * TU], in0=rts[b], scalar=1.0, in1=pzl[:, :], op0=ALU.add, op1=ALU.mult),
                      reads=[("rt", b), kzl], writes=[("szl", c % 2, t)])
                  for j in range(4):
                      S.op("tensor", lambda e, j=j: e.matmul(
                          pcv[:, :], lhsT=dgs[c % 2][:, j * 128:(j + 1) * 128], rhs=xl_[:, j:j + TU], start=(j == 0), stop=False),
                          reads=[("dg", c % 2), ("xlb", b), ("xlb", b, "h")], writes=[kcv])
                  S.op("tensor", lambda e: e.matmul(
                      pcv[:, :], lhsT=cbrow[c % 2], rhs=ones_bf[0:1, 0:TU] if False else onesrow, start=False, stop=True),
                      reads=[("cbrow", c % 2), ("onesrow",)], writes=[kcv])
                  S.op("scalar", lambda e: e.activation(out=xcbs[b], in_=pcv[:, :], func=AF.Copy),
                       reads=[kcv], writes=[("xcb", b)])

              def part2(c, t):
                  u = 4 * c + t
                  b = u % 2
                  pcv, kcv = ps[4 + b], ("ps", 4 + b)
                  rt_ = rts[b]
                  S.op("tensor", lambda e: e.matmul(ps[6][:, :], lhsT=wabs[c % 2], rhs=xcbs[b], start=True, stop=True),
                       reads=[("wab", c % 2), ("xcb", b)], writes=[("ps", 6)])
                  S.op("tensor", lambda e: e.matmul(ps[7][:, :], lhsT=wxbs[c % 2], rhs=xcbs[b], start=True, stop=True),
                       reads=[("wxb", c % 2), ("xcb", b)], writes=[("ps", 7)])
                  S.op("scalar", lambda e: e.activation(
                      out=rt_, in_=ps[6][:, :], func=AF.Tanh, scale=0.5, bias=habh[:, c:c + 1]),
                      reads=[("ps", 6), ("halfcols", "rab"), ("szl", c % 2, t)], writes=[("rt", b)])
                  S.op("scalar", lambda e: e.activation(
                      out=itv, in_=ps[7][:, :], func=AF.Tanh, scale=0.5, bias=hxbh[:, c:c + 1]),
                      reads=[("ps", 7), ("halfcols", "rxb")], writes=[("itv",)])
                  S.op("scalar", lambda e: e.activation(
                      out=Abig[:, t * TU:(t + 1) * TU], in_=rt_, func=AF.Exp, scale=hnsp[:, c:c + 1], bias=hnsp[:, c:c + 1]),
                      reads=[("rt", b), ("hnsp",)], writes=[("A", t)])
                  S.op("scalar", lambda e: e.activation(
                      out=SVbig[:, t * TU:(t + 1) * TU], in_=rt_, func=AF.Exp, scale=nsp[:, c:c + 1], bias=nsp[:, c:c + 1]),
                      reads=[("rt", b), ("nsp",)], writes=[("SV", t)])
                  S.op("vector", lambda e: e.scalar_tensor_tensor(
                      out=Mbig[:, t * TU:(t + 1) * TU], in0=itv, scalar=1.0, in1=pcv[:, :], op0=ALU.add, op1=ALU.mult),
                      reads=[("itv",), kcv], writes=[("M", t)])

              allk = lambda nm: [(nm, t) for t in range(4)]

              def stage_b(c):
                  for hf in range(2):
                      sl_ = slice(hf * 1024, (hf + 1) * 1024)
                      S.op("scalar", lambda e, sl_=sl_: e.activation(
                          out=SVbig[:, sl_], in_=SVbig[:, sl_], func=AF.Sqrt, scale=-0.25, bias=qcol),
                          reads=[("SV", 2 * hf), ("SV", 2 * hf + 1), ("qcol",)], writes=[("SV", 2 * hf), ("SV", 2 * hf + 1)])

              def stage_c(c, hf):
                  sl_ = slice(hf * 1024, (hf + 1) * 1024)
                  ks = lambda nm: [(nm, 2 * hf), (nm, 2 * hf + 1)]
                  S.op("vector", lambda e: e.tensor_tensor(
                      out=Mbig[:, sl_], in0=SVbig[:, sl_], in1=Mbig[:, sl_], op=ALU.mult),
                      reads=ks("SV") + ks("M"), writes=ks("M"))
                  if hf == 0:
                      S.op("vector", lambda e: e.tensor_tensor_scan(
                          out=SVbig[:, sl_], data0=Abig[:, sl_], data1=Mbig[:, sl_], initial=0.0,
                          op0=ALU.mult, op1=ALU.add),
                          reads=ks("A") + ks("M") + ks("SV"), writes=ks("SV"))
                      S.op("vector", lambda e: e.tensor_copy(out=hcar, in_=SVbig[:, 1023:1024]),
                           reads=ks("SV"), writes=[("hcar",)])
                  else:
                      S.op("vector", lambda e: e.tensor_tensor_scan(
                          out=SVbig[:, sl_], data0=Abig[:, sl_], data1=Mbig[:, sl_], initial=hcar,
                          op0=ALU.mult, op1=ALU.add),
                          reads=ks("A") + ks("M") + ks("SV") + [("hcar",)], writes=ks("SV"))
                  S.op("vector", lambda e: e.scalar_tensor_tensor(
                      out=cc[:, 4 + c, sl_], in0=SVbig[:, sl_], scalar=lnwh[:, c:c + 1], in1=szls[c % 2][:, sl_],
                      op0=ALU.mult, op1=ALU.mult),
                      reads=ks("SV") + [("szl", c % 2, 2 * hf), ("szl", c % 2, 2 * hf + 1), ("halfcols", "lnw")],
                      writes=[("cc", 4 + c, 2 * hf), ("cc", 4 + c, 2 * hf + 1)])
                  if hf == 1 and ("hl%d" % l) in dbg:
                      dump_part("hl%d" % l, c, 0, SVbig[:, 0:1024], allk("SV"))
                      dump_part("hl%d" % l, c, 1, SVbig[:, 1024:2048], allk("SV"))

              def stage_sq(c, hf):
                  sl_ = slice(hf * 1024, (hf + 1) * 1024)
                  S.op("scalar", lambda e: e.activation(out=hsq[:, sl_], in_=SVbig[:, sl_], func=AF.Square),
                       reads=[("SV", 2 * hf), ("SV", 2 * hf + 1)], writes=[("hsq", hf)])

              def stage_ssq(c):
                  for t16 in range(16):
                      S.op("tensor", lambda e, t16=t16: e.matmul(
                          ps[6][:, t16:t16 + 1], lhsT=hsq[:, t16 * 128:(t16 + 1) * 128], rhs=ones_bf[:, 0:1],
                          start=True, stop=True),
                          reads=[("hsq", 0), ("hsq", 1), ("ones_bf",)], writes=[("ps", 6)])
                  if c == 0:
                      S.op("vector", lambda e: e.tensor_copy(out=ssql, in_=ps[6][:, 0:16]),
                           reads=[("ps", 6)], writes=[("ssql", 0), ("ssql", 1)])
                  else:
                      S.op("vector", lambda e: e.tensor_tensor(out=ssql, in0=ssql, in1=ps[6][:, 0:16], op=ALU.add),
                           reads=[("ps", 6), ("ssql", 0), ("ssql", 1)], writes=[("ssql", 0), ("ssql", 1)])

              Vt = U[:, 14336:22656].rearrange("p (t h d) -> p t h d", t=16, h=8)
              lru_alias = ([("xlb", b_) for b_ in (0, 1)] + [("xlb", b_, "h") for b_ in (0, 1)] + [("xcb", b_) for b_ in (0, 1)]
                           + [("rt", b_) for b_ in (0, 1)] + [("itv",)] + [("dg", b_) for b_ in (0, 1)]
                           + [("wxb", b_) for b_ in (0, 1)] + [("cbrow", b_) for b_ in (0, 1)])

              def v_proj():
                  wv = stg[0][:, 0:4096].rearrange("p (k c) -> p k c", k=8)
                  S.op("gpsimd", lambda e: e.memset(Vt[:, :, :, 64:65], 1.0), writes=[("Vones",)] + lru_alias)
                  for tt in range(NT):
                      pv = ps[tt % 6]
                      for k in range(8):
                          S.op("tensor", lambda e, k=k, tt=tt, pv=pv: e.matmul(
                              pv[:, :], lhsT=hT[:, k, tt * 128:(tt + 1) * 128], rhs=wv[:, k, :], start=(k == 0), stop=(k == 7)),
                              reads=[("hT", tt), ("stg", 0)], writes=[("ps", tt % 6)])
                      S.op("scalar" if tt % 2 else "vector", (lambda e, tt=tt, pv=pv: e.activation(
                          out=Vt[:, tt, :, 0:64], in_=pv[:, :].rearrange("p (h d) -> p h d", h=8), func=AF.Copy)) if tt % 2 else
                          (lambda e, tt=tt, pv=pv: e.tensor_copy(
                              out=Vt[:, tt, :, 0:64], in_=pv[:, :].rearrange("p (h d) -> p h d", h=8))),
                          reads=[("ps", tt % 6)], writes=[("V", tt)] + lru_alias)

              units = [(c, t) for c in range(4) for t in range(4)]
              lru_load(0)
              lru_load(1)
              part1_mm(0, 0)
              part1_mm(0, 1)
              premm.update([(0, 0), (0, 1)])
              S.barrier()
              done1 = set()

              def p1(ui):
                  if ui < len(units) and ui not in done1:
                      done1.add(ui)
                      part1(*units[ui])

              p1(0)
              for ui, (c, t) in enumerate(units):
                  p1(ui + 1)
                  if c > 0 and t == 0:
                      stage_sq(c - 1, 0)
                  if c > 0 and t == 2:
                      stage_sq(c - 1, 1)
                      stage_ssq(c - 1)
                  part2(c, t)
                  if c > 0 and t == 0:
                      stage_c(c - 1, 1)
                  if c == 3 and t == 1:
                      S.dma("gpsimd", "stg0", stg[0][:, 0:4096].rearrange("p (k c) -> p k c", k=8), win_v[:, :, 1024:1536],
                            writes=[("stg", 0), ("stgz", 0)] + [("szl", 0, t_) for t_ in range(4)])
                  if t == 3:
                      stage_b(c)
                      p1(ui + 2)
                      stage_c(c, 0)
                      if c == 3:
                          stage_c(c, 1)
                          stage_sq(c, 0)
                          stage_sq(c, 1)
                          v_proj()
                      if c + 2 < 4:
                          lru_load(c + 2)
              stage_ssq(3)
              dump("cclru%d" % l, cc[:, 4:8, :], [128, 4, S_LEN], BF16,
                   [("cc", 4 + c, g) for c in range(4) for g in range(4)])
              dump("ssql%d" % l, ssql, [128, 16], F32, [("ssql", 0), ("ssql", 1)])
              S.barrier()
              if "stopP1" in dbg:
                  break

              qk = {("q", 0): U[:, 0:2048], ("k", 0): U[:, 2048:4096],
                    ("q", 1): U[:, 4096:6144], ("k", 1): U[:, 6144:8192]}
              szT = U[:, 8192:10240]
              gp = U[:, 10240:12288].rearrange("p (t f) -> p t f", t=16)
              PT = [mod_bc.bitcast(BF16)[:, i * 512:(i + 1) * 512] for i in range(4)]
              mt = U[:, 13312:13888].rearrange("p (j c) -> p j c", j=8)
              g8 = Uf[:, 6944:7008]
              top8 = Uf[:, 7008:7072]
              ltm = Uf[:, 7072:7136]
              kmf = Uf[:, 7136:7144]
              kmb = U[:, 14288:14296]
              junk64f = Uf[:, 11328:11392]
              ssqp = smc("ssqp", 128)
              rden = smc("rden8", 8)
              S.op("vector", lambda e: e.memset(qk[("q", 1)][0:64, :], 0.0), writes=[("qk", "q", 1, g) for g in range(4)])
              S.op("vector", lambda e: e.memset(qk[("k", 1)][0:64, :], 0.0), writes=[("qk", "k", 1, g) for g in range(4)])
              S.op("vector", lambda e: e.memset(qk[("q", 0)][64:72, :], 0.0), writes=[("qk", "q", 0, g) for g in range(4)])
              S.op("vector", lambda e: e.memset(U[:, 13312:13888], 0.0), writes=[("mt",)])
              S.dma("sync", "kr0", qk[("k", 0)][64:73, :], krows_d, writes=[("qk", "k", 0, g) for g in range(4)])
              S.dma("sync", "kr1", qk[("k", 1)][0:9, :], krows_d, writes=[("qk", "k", 1, g) for g in range(4)])
              S.dma("sync", "qo0", qk[("q", 0)][73:74, :], krows_d[8:9, :], writes=[("qk", "q", 0, g) for g in range(4)])
              S.dma("sync", "qo1", qk[("q", 1)][9:10, :], krows_d[8:9, :], writes=[("qk", "q", 1, g) for g in range(4)])
              S.dma("sync", "qo2", qk[("q", 0)][75:76, :], krows_d[8:9, :], writes=[("qk", "q", 0, g) for g in range(4)])
              S.dma("sync", "qo3", qk[("q", 1)][11:12, :], krows_d[8:9, :], writes=[("qk", "q", 1, g) for g in range(4)])
              S.dma("sync", "ko2", qk[("k", 0)][74:75, :], krows_d[8:9, :], writes=[("qk", "k", 0, g) for g in range(4)])
              S.dma("sync", "ko3", qk[("k", 1)][10:11, :], krows_d[8:9, :], writes=[("qk", "k", 1, g) for g in range(4)])
              wo_t = [stg[0][:, 0:4096].rearrange("p (c d) -> p c d", c=4), stg[1][:, 0:4096].rearrange("p (c d) -> p c d", c=4)]

              def wo_ap(ch, lo=0, hi=D):
                  return wo_t[ch // 4][:, ch % 4, lo:hi]

              def wo_load(c0, c1, extra_keys):
                  for ch in range(c0, c1):
                      S.dma("gpsimd", "wo%d" % (ch % 2), wo_ap(ch), wout_d[l, ch * 128:(ch + 1) * 128, :],
                            writes=[("wo", ch)] + extra_keys)

              def wo_scale(c0, c1):
                  for ch in range(c0, c1):
                      S.op("vector", lambda e, ch=ch: e.tensor_tensor(
                          out=wo_ap(ch), in0=wo_ap(ch), in1=mod_bc[:, 2048:3072], op=ALU.mult),
                          reads=[("wo", ch), ("mod", 4), ("mod", 5)], writes=[("wo", ch)])

              for p in range(4):
                  wp = stg[1][:, 0:3072].rearrange("p (k c) -> p k c", k=8)
                  S.dma("gpsimd", "stg1", wp[:, :, 0:128], win_v[:, :, p * 128:(p + 1) * 128], writes=[("stgp", 0)])
                  S.dma("gpsimd", "stg1b", wp[:, :, 128:256], win_v[:, :, 512 + p * 128:512 + (p + 1) * 128], writes=[("stgp", 1)])
                  S.dma("gpsimd", "stg1c", wp[:, :, 256:384], win_v[:, :, 1536 + p * 128:1536 + (p + 1) * 128], writes=[("stgp", 2)])
                  S.dma("sync", "ar0", qk[("q", 0)][72:73, :], arow_d[2 * p:2 * p + 1, :], writes=[("qk", "q", 0, g) for g in range(4)])
                  S.dma("sync", "ar1", qk[("q", 1)][8:9, :], arow_d[2 * p + 1:2 * p + 2, :], writes=[("qk", "q", 1, g) for g in range(4)])
                  S.dma("sync", "ka0", qk[("k", 0)][73:74, :], karow_d[2 * p:2 * p + 1, :], writes=[("qk", "k", 0, g) for g in range(4)])
                  S.dma("sync", "ka1", qk[("k", 1)][9:10, :], karow_d[2 * p + 1:2 * p + 2, :], writes=[("qk", "k", 1, g) for g in range(4)])
                  S.dma("sync", "ah0", qk[("q", 0)][74:75, :], arowhi_d[2 * p:2 * p + 1, :], writes=[("qk", "q", 0, g) for g in range(4)])
                  S.dma("sync", "ah1", qk[("q", 1)][10:11, :], arowhi_d[2 * p + 1:2 * p + 2, :], writes=[("qk", "q", 1, g) for g in range(4)])
                  S.dma("sync", "kh0", qk[("k", 0)][75:76, :], karowhi_d[2 * p:2 * p + 1, :], writes=[("qk", "k", 0, g) for g in range(4)])
                  S.dma("sync", "kh1", qk[("k", 1)][11:12, :], karowhi_d[2 * p + 1:2 * p + 2, :], writes=[("qk", "k", 1, g) for g in range(4)])
                  pi_ = [0]

                  PB = [0, 1, 4, 5]

                  def proj(which, wi, tg):
                      pq = ps[PB[pi_[0] % 4]]
                      pkey = ("ps", PB[pi_[0] % 4])
                      pi_[0] += 1
                      hkeys = [("hT", 4 * tg + j) for j in range(4)]
                      for k in range(8):
                          S.op("tensor", lambda e, k=k: e.matmul(
                              pq[:, :], lhsT=wp[:, k, wi * 128:(wi + 1) * 128], rhs=hT[:, k, tg * 512:(tg + 1) * 512],
                              start=(k == 0), stop=(k == 7)),
                              reads=hkeys + [("stgp", wi)], writes=[pkey])
                      if which in "qk":
                          S.op("vector", lambda e: e.tensor_copy(
                              out=qk[(which, 0)][0:64, tg * 512:(tg + 1) * 512], in_=pq[0:64, :]),
                              reads=[pkey], writes=[("qk", which, 0, tg)])
                          S.op("scalar", lambda e: e.activation(
                              out=qk[(which, 1)][64:128, tg * 512:(tg + 1) * 512], in_=pq[64:128, :], func=AF.Copy),
                              reads=[pkey], writes=[("qk", which, 1, tg)])
                      else:
                          S.op("scalar", lambda e: e.activation(
                              out=szT[:, tg * 512:(tg + 1) * 512], in_=pq[:, :], func=AF.Tanh, scale=0.5),
                              reads=[pkey], writes=[("szT", tg)])
                          S.op("vector", lambda e: e.scalar_tensor_tensor(
                              out=szT[:, tg * 512:(tg + 1) * 512], in0=szT[:, tg * 512:(tg + 1) * 512], scalar=1.0,
                              in1=pq[:, :], op0=ALU.add, op1=ALU.mult),
                              reads=[pkey, ("szT", tg)], writes=[("szT", tg)])

                  def kmean(hb):
                      r0 = 64 * hb
                      kh = qk[("k", hb)]
                      kkeys = [("qk", "k", hb, g) for g in range(4)]
                      S.op("vector", lambda e: e.tensor_reduce(
                          out=kmf[r0:r0 + 64, :], in_=kh[r0:r0 + 64, :].rearrange("p (n t) -> p n t", n=8), axis=AX.X, op=ALU.add),
                          reads=kkeys, writes=[("kmf", hb)])
                      S.op("vector", lambda e: e.tensor_scalar(
                          out=kmb[r0:r0 + 64, :], in0=kmf[r0:r0 + 64, :], scalar1=1.0 / 256, scalar2=None, op0=ALU.mult),
                          reads=[("kmf", hb)], writes=[("kmb", hb)])

                  def gates(hb):
                      r0 = 64 * hb
                      qh = qk[("q", hb)]
                      for j in range(8):
                          S.op("tensor", lambda e, j=j: e.matmul(
                              ps[2][:, j * 8:(j + 1) * 8], lhsT=qh[r0:r0 + 64, 1024 + j * 128:1024 + (j + 1) * 128],
                              rhs=kmb[r0:r0 + 64, :], start=True, stop=True),
                              reads=[("qk", "q", hb, 2 + j // 4), ("kmb", hb)], writes=[("ps", 2)])

                  def topk(hb):
                      S.op("vector", lambda e: e.tensor_tensor(out=g8, in0=ps[2][:, 0:64], in1=gmask[:, :], op=ALU.add),
                           reads=[("ps", 2), ("gmask",)], writes=[("g8",)])
                      for j in range(8):
                          S.op("vector", lambda e, j=j: e.max(out=top8[:, j * 8:(j + 1) * 8], in_=g8[:, j * 8:(j + 1) * 8]),
                               reads=[("g8",)], writes=[("top8", j)])
                      thr_b = bass.AP(top8.tensor, top8.offset + 2, [[top8.ap[0][0], 128], [8, 8], [0, 8]])
                      S.op("vector", lambda e: e.tensor_tensor(
                          out=ltm.rearrange("p (j n) -> p j n", j=8), in0=g8.rearrange("p (j n) -> p j n", j=8),
                          in1=thr_b, op=ALU.is_lt),
                          reads=[("g8",)] + [("top8", j) for j in range(8)], writes=[("ltm",)])
                      S.op("vector", lambda e: e.tensor_tensor(
                          out=mt[:, :, 64:72], in0=ltm.rearrange("p (j n) -> p j n", j=8),
                          in1=vneg[:, :].rearrange("p (j n) -> p j n", j=8), op=ALU.mult),
                          reads=[("ltm",), ("vneg",)], writes=[("mt",)])

                  def masks_T(hb):
                      qh = qk[("q", hb)]
                      a0 = 64 if hb == 0 else 0
                      for j in range(8):
                          if hb == 0:
                              S.op("tensor", lambda e, j=j: e.transpose(
                                  out=psb[3][0:72, j * 128:(j + 1) * 128], in_=mt[:, j, 0:72], identity=ident_bf[:, :]),
                                  reads=[("mt",), ("ident_bf",)], writes=[("ps", 3)])
                          else:
                              S.op("tensor", lambda e, j=j: e.transpose(
                                  out=psb[3][0:8, j * 128:(j + 1) * 128], in_=mt[:, j, 64:72], identity=ident_bf[:, :]),
                                  reads=[("mt",), ("ident_bf",)], writes=[("ps", 3)])
                      S.op("vector", lambda e: e.tensor_copy(
                          out=qh[a0:a0 + 8, 1024:2048], in_=psb[3][a0:a0 + 8, 0:1024]),
                          reads=[("ps", 3)], writes=[("qk", "q", hb, 2), ("qk", "q", hb, 3)])

                  if p == 1:
                      wo_load(0, 4, [("stg", 0)])
                  if p == 2:
                      wo_scale(0, 4)
                  for tg in range(4):
                      proj("k", 1, tg)
                  kmean(0)
                  kmean(1)
                  for tg in range(4):
                      proj("q", 0, tg)
                  gates(0)
                  topk(0)
                  proj("z", 2, 0)
                  proj("z", 2, 1)
                  masks_T(0)
                  gates(1)
                  topk(1)
                  proj("z", 2, 2)
                  proj("z", 2, 3)
                  masks_T(1)
                  if p == 3:
                      wo_load(4, 8, [("stgp", 0), ("stgp", 1), ("stgp", 2)])
                  stop_at("stopProj")
                  stop_at("stopMask")
                  for hb in range(2):
                      h = 2 * p + hb
                      r0 = 64 * hb
                      qh = qk[("q", hb)]
                      kh = qk[("k", hb)]
                      K1 = 76 if hb == 0 else 128
                      steps = [(G, kt) for G in range(4) for kt in range(4 * G + 4)]
                      LOOK = 3
                      SB = [4, 5, 0, 1]

                      def geom(i):
                          G, kt = steps[i]
                          rel = kt // 2 - 2 * G
                          if rel < 0:
                              return G, kt, 0, None
                          c0 = 256 * rel + 128 * (kt % 2)
                          return G, kt, c0, c0

                      def emit_S(i, qh=qh, kh=kh, K1=K1, hb=hb):
                          G, kt, c0, tri = geom(i)
                          st_, skey = ps[SB[i % 4]], ("ps", SB[i % 4])
                          S.op("tensor", lambda e: e.matmul(
                              st_[:, c0:512], lhsT=kh[0:K1, kt * 128:(kt + 1) * 128],
                              rhs=qh[0:K1, G * 512 + c0:(G + 1) * 512], start=True, stop=(tri is None)),
                              reads=[("qk", "k", hb, kt // 4), ("qk", "q", hb, G)], writes=[skey])
                          if tri is not None:
                              S.op("tensor", lambda e: e.matmul(
                                  st_[:, tri:tri + 128], lhsT=ident_bf[:, :], rhs=trimask[:, :], start=False, stop=True),
                                  reads=[("ident_bf",), ("trimask",)], writes=[skey])

                      def emit_E(i):
                          G, kt, c0, tri = geom(i)
                          st_, skey = ps[SB[i % 4]], ("ps", SB[i % 4])
                          S.op("scalar", lambda e: e.activation(
                              out=PT[i % 4][:, c0:512], in_=st_[:, c0:512], func=AF.Exp, scale=0.125),
                              reads=[skey], writes=[("PT", i % 4)])

                      def emit_PV(i, h=h, hb=hb):
                          G, kt, c0, tri = geom(i)
                          accb = ps[6 + G % 2]
                          akey = ("acc", G % 2)
                          for qt in range(c0 // 128, 4):
                              lastkt = 4 * G + qt
                              S.op("tensor", lambda e, qt=qt: e.matmul(
                                  accb[:, qt * 128:qt * 128 + 65], lhsT=PT[i % 4][:, qt * 128:(qt + 1) * 128],
                                  rhs=Vt[:, kt, h, :], start=(kt == 0 and qt == 0), stop=(kt == lastkt),
                                  skip_group_check=True),
                                  reads=[("PT", i % 4), ("V", kt), ("Vones",)], writes=[akey])
                          if kt == 4 * G + 3:
                              rd = rden[:, 4 * (G % 2):4 * (G % 2) + 4]
                              S.op("vector", lambda e: e.reciprocal(
                                  out=rd, in_=accb[:, 0:512].rearrange("p (t c) -> p t c", t=4)[:, :, 64]),
                                  reads=[akey], writes=[("rden", G % 2)])
                              for qt in range(4):
                                  tt = 4 * G + qt
                                  S.op("vector", lambda e, qt=qt, tt=tt: e.tensor_scalar(
                                      out=gp[:, tt, 64 * hb:64 * hb + 64], in0=accb[:, qt * 128:qt * 128 + 64],
                                      scalar1=rd[:, qt:qt + 1], scalar2=None, op0=ALU.mult),
                                      reads=[akey, ("rden", G % 2)], writes=[("gp", tt, hb)])
                                  S.op("vector", lambda e, qt=qt, tt=tt: e.scalar_tensor_tensor(
                                      out=junk64f, in0=accb[:, qt * 128:qt * 128 + 64], scalar=rd[:, qt:qt + 1],
                                      in1=gp[:, tt, 64 * hb:64 * hb + 64], op0=ALU.mult, op1=ALU.mult,
                                      accum_out=ssqp[:, h * 16 + tt:h * 16 + tt + 1]),
                                      reads=[akey, ("rden", G % 2), ("gp", tt, hb)], writes=[("junk64",), ("ssqp", h, tt)])

                      for i in range(len(steps) + LOOK):
                          if i < len(steps):
                              emit_S(i)
                          j = i - LOOK
                          if j >= 0:
                              emit_E(j)
                              emit_PV(j)
                  stop_at("stopAttnPair")
                  for tg in range(4):
                      tb = 3 if tg % 2 == 0 else 2
                      for j in range(4):
                          tt = 4 * tg + j
                          S.op("tensor", lambda e, tt=tt, j=j, tb=tb: e.transpose(
                              out=psb[tb][:, j * 128:(j + 1) * 128], in_=gp[:, tt, :], identity=ident_bf[:, :]),
                              reads=[("gp", tt, 0), ("gp", tt, 1), ("ident_bf",)], writes=[("ps", tb)])
                      S.op("vector", lambda e, tg=tg, p=p, tb=tb: e.scalar_tensor_tensor(
                          out=cc[:, p, tg * 512:(tg + 1) * 512], in0=psb[tb][:, 0:512], scalar=anwh[:, p:p + 1],
                          in1=szT[:, tg * 512:(tg + 1) * 512], op0=ALU.mult, op1=ALU.mult),
                          reads=[("ps", tb), ("szT", tg), ("halfcols", "anw")], writes=[("cc", p, tg)])
              ssqa = smc("ssqa", 16)
              S.op("vector", lambda e: e.tensor_reduce(
                  out=ssqa, in_=ssqp.rearrange("p (h t) -> p t h", h=8), axis=AX.X, op=ALU.add),
                  reads=[("ssqp", h_, t_) for h_ in range(8) for t_ in range(16)], writes=[("ssqa",)])
              dump("ccattn%d" % l, cc[:, 0:4, :], [128, 4, S_LEN], BF16,
                   [("cc", c, g) for c in range(4) for g in range(4)])
              dump("ssqa%d" % l, ssqa, [128, 16], F32, [("ssqa",)])
              S.barrier()
              if "stopP2" in dbg:
                  break

              fnw_bc = Uf[:, 4096:5120]
              outt = [Uf[:, 5120:6144], Uf[:, 6144:7168]]
              junk3 = U[:, 14336:15360]
              rstda = smc("rstda", 16)
              rstdl = smc("rstdl", 16)
              for (dst, src_, key) in ((rstda, ssqa, ("ssqa",)), (rstdl, ssql, None)):
                  rk = [key] if key else [("ssql", 0), ("ssql", 1)]
                  S.op("gpsimd", lambda e, dst=dst, src_=src_: e.tensor_scalar(
                      out=dst, in0=src_, scalar1=1.0 / 512, scalar2=EPS, op0=ALU.mult, op1=ALU.add),
                      reads=rk, writes=[("rstd", id(dst))])
                  S.op("gpsimd", lambda e, dst=dst: e.tensor_tensor(
                      out=dst, in0=dst, in1=bass.AP(mhalf.tensor, mhalf.offset, [[mhalf.ap[0][0], 128], [0, 16]]), op=ALU.pow),
                      reads=[("rstd", id(dst)), ("mhalf",)], writes=[("rstd", id(dst))])
              rstd_keys = [("rstd", id(rstda)), ("rstd", id(rstdl))]
              wo_scale(4, 8)
              if final and last_layer:
                  fsrc = bass.AP(fnw_d.tensor, 0, [[0, 128], [1, D]])
                  S.dma("sync", "fnw", fnw_bc, fsrc, writes=[("fnw",)])
                  ssqf = smc("ssqf", 16)
                  rstdf = smc("rstdf", 16)
              def final_out(t_):
                  ot = outt[t_ % 2]
                  S.op("vector", lambda e: e.scalar_tensor_tensor(
                      out=ot, in0=x_sb[:, t_, :], scalar=rstdf[:, t_:t_ + 1], in1=fnw_bc, op0=ALU.mult, op1=ALU.mult),
                      reads=[("x", t_), ("rstdf", t_), ("fnw",)], writes=[("outt", t_ % 2)])
                  tok = S.dma("sync", "xout%d" % (t_ % 2), out_d[t_ * 128:(t_ + 1) * 128, :], ot,
                              reads=[("outt", t_ % 2)])
                  S.wait_at_end("sync", tok)

              cckeys = lambda tt: [("cc", ch, tt // 4) for ch in range(8)]
              it = 0
              nxt = (not last_layer)
              if nxt:
                  mprep_n = mod_prep(16384, 17408)
              for tt in range(NT):
                  if nxt and tt in (0, 2, 6, 8, 10, 12):
                      mod_dma(layers[li + 1], (0, 2, 6, 8, 10, 12).index(tt), True)
                  if nxt and tt % 2 == 1 and tt >= 5 and (tt - 5) // 2 < 6:
                      mod_mm(layers[li + 1], (tt - 5) // 2, mprep_n, True)
                  for dh in range(2):
                      nset = 2 if nxt else 3
                      pa = ps[2 * (it % nset)]
                      pl = ps[2 * (it % nset) + 1]
                      ka = ("ps", 2 * (it % nset))
                      kl = ("ps", 2 * (it % nset) + 1)
                      it += 1
                      for ch in range(4):
                          S.op("tensor", lambda e, ch=ch, tt=tt, dh=dh, pa=pa: e.matmul(
                              pa[:, :], lhsT=cc[:, ch, tt * 128:(tt + 1) * 128], rhs=wo_ap(ch, dh * 512, (dh + 1) * 512),
                              start=(ch == 0), stop=(ch == 3)),
                              reads=[("cc", ch, tt // 4), ("wo", ch)], writes=[ka])
                      for ch in range(4, 8):
                          S.op("tensor", lambda e, ch=ch, tt=tt, dh=dh, pl=pl: e.matmul(
                              pl[:, :], lhsT=cc[:, ch, tt * 128:(tt + 1) * 128], rhs=wo_ap(ch, dh * 512, (dh + 1) * 512),
                              start=(ch == 4), stop=(ch == 7)),
                              reads=[("cc", ch, tt // 4), ("wo", ch)], writes=[kl])
                      xs = x_sb[:, tt, dh * 512:(dh + 1) * 512]
                      S.op("vector", lambda e, xs=xs, pa=pa, tt=tt: e.scalar_tensor_tensor(
                          out=xs, in0=pa[:, :], scalar=rstda[:, tt:tt + 1], in1=xs, op0=ALU.mult, op1=ALU.add),
                          reads=[ka, ("x", tt)] + rstd_keys, writes=[("x", tt)])
                      S.op("vector", lambda e, xs=xs, pl=pl, tt=tt: e.scalar_tensor_tensor(
                          out=xs, in0=pl[:, :], scalar=rstdl[:, tt:tt + 1], in1=xs, op0=ALU.mult, op1=ALU.add),
                          reads=[kl, ("x", tt)] + rstd_keys, writes=[("x", tt)])
                  if nxt:
                      x_stats(tt, junk3)
                  if final and last_layer:
                      S.op("scalar", lambda e, tt=tt: e.activation(
                          out=junk3, in_=x_sb[:, tt, :], func=AF.Square, accum_out=ssqf[:, tt:tt + 1]),
                          reads=[("x", tt)], writes=[("junk3",), ("ssqf", tt)])
                      S.op("gpsimd", lambda e, tt=tt: e.tensor_scalar(
                          out=rstdf[:, tt:tt + 1], in0=ssqf[:, tt:tt + 1], scalar1=1.0 / D, scalar2=EPS,
                          op0=ALU.mult, op1=ALU.add),
                          reads=[("ssqf", tt)], writes=[("rstdf", tt)])
                      S.op("gpsimd", lambda e, tt=tt: e.tensor_tensor(
                          out=rstdf[:, tt:tt + 1], in0=rstdf[:, tt:tt + 1], in1=mhalf, op=ALU.pow),
                          reads=[("rstdf", tt), ("mhalf",)], writes=[("rstdf", tt)])
                      if tt >= 2:
                          final_out(tt - 2)
              if final and last_layer:
                  final_out(NT - 2)
                  final_out(NT - 1)
              elif last_layer:
                  for tt in range(NT):
                      tok = S.dma("sync", "xout%d" % (tt % 4), out_d[tt * 128:(tt + 1) * 128, :], x_sb[:, tt, :],
                                  reads=[("x", tt)])
                      S.wait_at_end("sync", tok)
              dump("xend%d" % l, x_sb[:, :, :], [128, NT, D], F32, [("x", t) for t in range(NT)])
              S.barrier()


        except _Stop:
            pass

        for sname, n in S.dma_cnt.items():
            S.wait_at_end("sync", ("d_" + sname, 16 * n))
        run = S.emit_all(None)
        with nc.Block() as block:
            @block.sync
            def _(e):
                run("sync", e)

            @block.scalar
            def _(e):
                run("scalar", e)

            @block.vector
            def _(e):
                run("vector", e)

            @block.gpsimd
            def _(e):
                run("gpsimd", e)

            @block.tensor
            def _(e):
                run("tensor", e)
    return nc, list(dbg_d.keys())


def make_inmaps(inp, L_total, x_override=None):
    consts = host_consts()
    inp = {k: np.asarray(v) for k, v in inp.items()}
    rab = blockdiag(inp["rg_a_w"].astype(np.float32))
    rxb = blockdiag(inp["rg_x_w"].astype(np.float32))
    xs = inp["x"] if x_override is None else x_override
    maps = []
    for b in range(8):
        m = {
            "x": np.ascontiguousarray(xs[b], dtype=np.float32),
            "cols": pack_cols(inp, b, L_total),
            "ada_w": np.ascontiguousarray(inp["ada_w"], dtype=np.float32),
            "ada_b": np.ascontiguousarray(inp["ada_b"], dtype=np.float32),
            "w_in": np.ascontiguousarray(inp["w_in"], dtype=np.float32),
            "w_out": np.ascontiguousarray(inp["w_out"], dtype=np.float32),
            "rg_a_bd": rab,
            "rg_x_bd": rxb,
            "final_norm_w": np.ascontiguousarray(inp["final_norm_w"], dtype=np.float32),
            "conv_b": np.ascontiguousarray(inp["conv_b"], dtype=np.float32),
        }
        m.update(consts)
        maps.append(m)
    return maps


_CACHE = {}


def kernel(**inputs):
    L_total = 2
    maps = make_inmaps(inputs, L_total)
    key = ("full",)
    if key not in _CACHE:
        _CACHE[key] = build([0, 1], True, L_total)[0]
    nc = _CACHE[key]
    res = run_bass_kernel_spmd(nc, maps, core_ids=list(range(8)))
    out = np.stack([np.asarray(r["out"], dtype=np.float32) for r in res.results], axis=0)
    return out
```
